# Optimizing a Trainium2 kernel written in Bass

```python
import jax, jax.numpy as jnp
from jax import lax
import numpy as np

D_MODEL = 1024
BATCH = 8
SEQ = 8192
DEPTH = 1

ATTN_HEADS = 8
ATTN_KV_HEADS = 2
ATTN_HEAD_DIM = 64
ATTN_W = ATTN_HEADS * ATTN_HEAD_DIM
IDX_HEADS = 8
IDX_DIM = 64
TOPK_MAX = 256
Q_BLOCK = 128
GLA_HEADS = 4
GLA_DK = 64
GLA_DV = 128
GLA_W = GLA_HEADS * GLA_DV
GLA_GATE_RANK = 16
GLA_TAU = 16.0
GLA_CHUNK = 64
MIX_W = ATTN_W + GLA_W
D_FF = 2816
EPS = 1e-6

IN_SPLITS = (
    ATTN_HEADS * ATTN_HEAD_DIM,
    ATTN_KV_HEADS * ATTN_HEAD_DIM,
    ATTN_KV_HEADS * ATTN_HEAD_DIM,
    IDX_HEADS * IDX_DIM,
    IDX_DIM,
    IDX_HEADS,
    GLA_HEADS * GLA_DK,
    GLA_HEADS * GLA_DK,
    GLA_HEADS * GLA_DV,
    GLA_GATE_RANK,
    GLA_HEADS * GLA_DV,
)
IN_COLS = sum(IN_SPLITS)

kernel_name = "hybrid_dsa_gla_macaron_sandwich"


def rmsnorm(x, g):
    xf = x.astype(jnp.float32)
    y = xf * lax.rsqrt(jnp.mean(xf * xf, axis=-1, keepdims=True) + EPS)
    return (y * g.astype(jnp.float32)).astype(x.dtype)


def swiglu(x, w_gate, w_up, w_down):
    return (jax.nn.silu(x @ w_gate) * (x @ w_up)) @ w_down


def dsa_attention(q, k, v, iq, ik, iw):
    B, S = q.shape[0], q.shape[1]
    topk = min(TOPK_MAX, S // 4)
    nb = S // Q_BLOCK
    rep = ATTN_HEADS // ATTN_KV_HEADS
    key_pos = jnp.arange(S, dtype=jnp.int32)

    def to_blocks(t):
        return t.reshape((B, nb, Q_BLOCK) + t.shape[2:]).swapaxes(0, 1)

    def one_block(args):
        qb, iqb, iwb, q0 = args
        qpos = q0 + jnp.arange(Q_BLOCK, dtype=jnp.int32)
        causal = key_pos[None, :] <= qpos[:, None]
        logits = jnp.einsum('bthd,bsd->bths', iqb, ik,
                            preferred_element_type=jnp.float32) * (IDX_DIM ** -0.5)
        score = jnp.einsum('bths,bth->bts', jax.nn.relu(logits), iwb.astype(jnp.float32))
        score = jnp.where(causal[None], score, -jnp.inf)
        _, idx = lax.top_k(score, topk)
        valid = idx <= qpos[None, :, None]
        ks = jax.vmap(lambda kk, ii: kk[ii])(k, idx)
        vs = jax.vmap(lambda vv, ii: vv[ii])(v, idx)
        qg = qb.reshape(B, Q_BLOCK, ATTN_KV_HEADS, rep, ATTN_HEAD_DIM)
        s = jnp.einsum('btgrd,btkgd->btgrk', qg, ks,
                       preferred_element_type=jnp.float32) * (ATTN_HEAD_DIM ** -0.5)
        s = jnp.where(valid[:, :, None, None, :], s, -jnp.inf)
        p = jax.nn.softmax(s, axis=-1).astype(vs.dtype)
        o = jnp.einsum('btgrk,btkgd->btgrd', p, vs)
        return o.reshape(B, Q_BLOCK, ATTN_W)

    q_starts = jnp.arange(nb, dtype=jnp.int32) * Q_BLOCK
    out = lax.map(one_block, (to_blocks(q), to_blocks(iq), to_blocks(iw), q_starts))
    return out.swapaxes(0, 1).reshape(B, S, ATTN_W)


def gla(q, k, v, log_a, g, norm_g):
    B, S = q.shape[0], q.shape[1]
    nc = S // GLA_CHUNK
    out_dtype = v.dtype

    def chunk(t):
        return t.astype(jnp.float32).reshape(B, nc, GLA_CHUNK, GLA_HEADS, -1).transpose(0, 3, 1, 2, 4)

    qc = chunk(q) * (GLA_DK ** -0.5)
    kc, vc, ac = chunk(k), chunk(v), chunk(log_a)
    b = jnp.cumsum(ac, axis=3)
    b_last = b[:, :, :, -1:, :]
    q_dec = qc * jnp.exp(b)
    k_in = kc * jnp.exp(-b)
    k_out = kc * jnp.exp(b_last - b)
    tri = jnp.tril(jnp.ones((GLA_CHUNK, GLA_CHUNK), dtype=bool))
    A = jnp.where(tri, jnp.einsum('bhncd,bhnsd->bhncs', q_dec, k_in), 0.0)
    o_intra = jnp.einsum('bhncs,bhnsv->bhncv', A, vc)
    upd = jnp.einsum('bhnsd,bhnsv->bhndv', k_out, vc)
    decay = jnp.exp(b_last[:, :, :, 0, :])

    def step(state, inp):
        dec, u = inp
        return dec[..., None] * state + u, state

    init = jnp.zeros((B, GLA_HEADS, GLA_DK, GLA_DV), jnp.float32)
    _, s_prev = lax.scan(step, init, (decay.transpose(2, 0, 1, 3), upd.transpose(2, 0, 1, 3, 4)))
    s_prev = s_prev.transpose(1, 2, 0, 3, 4)
    o_inter = jnp.einsum('bhncd,bhndv->bhncv', q_dec, s_prev)
    o = (o_intra + o_inter).transpose(0, 2, 3, 1, 4).reshape(B, S, GLA_HEADS, GLA_DV)
    o = rmsnorm(o, norm_g).reshape(B, S, GLA_W) * jax.nn.silu(g.astype(jnp.float32))
    return o.astype(out_dtype)


def hybrid_mixer(h, w_in, w_gla_a2, b_gla_a, g_gla_norm, w_out):
    B, S = h.shape[0], h.shape[1]
    proj = h @ w_in
    offsets = [int(o) for o in np.cumsum(IN_SPLITS)[:-1]]
    (aq, ak, av, iq, ik, iw, gq, gk, gv, ga, gg) = jnp.split(proj, offsets, axis=-1)
    aq = aq.reshape(B, S, ATTN_HEADS, ATTN_HEAD_DIM)
    ak = ak.reshape(B, S, ATTN_KV_HEADS, ATTN_HEAD_DIM)
    av = av.reshape(B, S, ATTN_KV_HEADS, ATTN_HEAD_DIM)
    iq = iq.reshape(B, S, IDX_HEADS, IDX_DIM)
    iw = iw * (IDX_HEADS ** -0.5)
    o_attn = dsa_attention(aq, ak, av, iq, ik, iw)
    gate_logit = (ga @ w_gla_a2 + b_gla_a).astype(jnp.float32)
    log_a = jax.nn.log_sigmoid(gate_logit) / GLA_TAU
    o_gla = gla(gq.reshape(B, S, GLA_HEADS, GLA_DK), gk.reshape(B, S, GLA_HEADS, GLA_DK),
                gv.reshape(B, S, GLA_HEADS, GLA_DV), log_a.reshape(B, S, GLA_HEADS, GLA_DK),
                gg, g_gla_norm)
    return jnp.concatenate([o_attn, o_gla.astype(o_attn.dtype)], axis=-1) @ w_out


def setup_inputs(seed: int = 0) -> dict:
    key = jax.random.key(seed)
    ks = jax.random.split(key, 24)
    f32 = jnp.float32

    def w(k, shape, fan_in):
        return jax.random.normal(k, shape, f32) * (fan_in ** -0.5)

    def gain(k, n):
        return 1.0 + 0.05 * jax.random.normal(k, (DEPTH, n), f32)

    return {
        "x": jax.random.normal(ks[0], (BATCH, SEQ, D_MODEL), f32),
        "g_ffn1_pre": gain(ks[1], D_MODEL),
        "w_ffn1_gate": w(ks[2], (DEPTH, D_MODEL, D_FF), D_MODEL),
        "w_ffn1_up": w(ks[3], (DEPTH, D_MODEL, D_FF), D_MODEL),
        "w_ffn1_down": w(ks[4], (DEPTH, D_FF, D_MODEL), D_FF),
        "g_ffn1_post": gain(ks[5], D_MODEL),
        "g_mix_pre": gain(ks[6], D_MODEL),
        "w_in": w(ks[7], (DEPTH, D_MODEL, IN_COLS), D_MODEL),
        "w_gla_a2": w(ks[8], (DEPTH, GLA_GATE_RANK, GLA_HEADS * GLA_DK), GLA_GATE_RANK),
        "b_gla_a": 0.1 * jax.random.normal(ks[9], (DEPTH, GLA_HEADS * GLA_DK), f32),
        "g_gla_norm": gain(ks[10], GLA_DV),
        "w_out": w(ks[11], (DEPTH, MIX_W, D_MODEL), MIX_W),
        "g_mix_post": gain(ks[12], D_MODEL),
        "g_ffn2_pre": gain(ks[13], D_MODEL),
        "w_ffn2_gate": w(ks[14], (DEPTH, D_MODEL, D_FF), D_MODEL),
        "w_ffn2_up": w(ks[15], (DEPTH, D_MODEL, D_FF), D_MODEL),
        "w_ffn2_down": w(ks[16], (DEPTH, D_FF, D_MODEL), D_FF),
        "g_ffn2_post": gain(ks[17], D_MODEL),
    }


def reference(x, g_ffn1_pre, w_ffn1_gate, w_ffn1_up, w_ffn1_down, g_ffn1_post,
              g_mix_pre, w_in, w_gla_a2, b_gla_a, g_gla_norm, w_out, g_mix_post,
              g_ffn2_pre, w_ffn2_gate, w_ffn2_up, w_ffn2_down, g_ffn2_post):
    for l in range(DEPTH):
        f = swiglu(rmsnorm(x, g_ffn1_pre[l]), w_ffn1_gate[l], w_ffn1_up[l], w_ffn1_down[l])
        x = x + 0.5 * rmsnorm(f, g_ffn1_post[l])
        m = hybrid_mixer(rmsnorm(x, g_mix_pre[l]), w_in[l], w_gla_a2[l], b_gla_a[l],
                         g_gla_norm[l], w_out[l])
        x = x + rmsnorm(m, g_mix_post[l])
        f = swiglu(rmsnorm(x, g_ffn2_pre[l]), w_ffn2_gate[l], w_ffn2_up[l], w_ffn2_down[l])
        x = x + 0.5 * rmsnorm(f, g_ffn2_post[l])
    return x
```

```python
import numpy as np
from contextlib import ExitStack
import concourse.bass as bass
import concourse.mybir as mybir
from concourse.bass_utils import run_bass_kernel_spmd

F32 = mybir.dt.float32
BF16 = mybir.dt.bfloat16
AF = mybir.ActivationFunctionType
ALU = mybir.AluOpType
AX = mybir.AxisListType

D = 1024
DFF = 2816
NFF = DFF // 128
KC = D // 128
EPS = 1e-6
SEQ = 8192
NCORES = 8


class Buf:
    __slots__ = ("w", "r", "x")

    def __init__(self, x=False):
        self.w = None
        self.r = {}
        self.x = x


class Eng:
    def __init__(self, S, name, eng, strict):
        self.S = S
        self.name = name
        self.eng = eng
        self.strict = strict
        self.sem = S.new_sem(name)
        self.count = 0
        self.seen = {}
        self.slots = None
        self.slot_i = 0


class Sync:
    SEM_ROLL = 30000
    NSLOT = 8

    def __init__(self, nc, es):
        self.nc = nc
        self.es = es
        self.nsem = 0
        self.pe = Eng(self, "pe", nc.tensor, False)
        self.act = Eng(self, "act", nc.scalar, True)
        self.dve = Eng(self, "dve", nc.vector, True)
        self.pool = Eng(self, "pool", nc.gpsimd, True)
        self.sp = Eng(self, "sp", nc.sync, False)
        self.engs = [self.pe, self.act, self.dve, self.pool, self.sp]
        for q in (self.sp, self.pool, self.act):
            q.slots = [[self.new_sem(q.name + "d%d" % i), 0] for i in range(self.NSLOT)]

    def new_sem(self, name):
        self.nsem += 1
        return self.es.enter_context(self.nc.semaphore("s%d_%s" % (self.nsem, name)))

    def _wait(self, E, tok):
        sem, val, owner = tok
        if owner is E and not E.strict:
            return
        k = id(sem)
        if E.seen.get(k, 0) >= val:
            return
        E.eng.wait_ge(sem, val)
        E.seen[k] = val

    def _deps(self, E, reads, writes):
        for b in reads:
            if b.w is not None:
                self._wait(E, b.w)
            if b.x:
                for tok in b.r.values():
                    if tok[2] is not E:
                        self._wait(E, tok)
        for b in writes:
            if b.w is not None:
                self._wait(E, b.w)
            for tok in b.r.values():
                self._wait(E, tok)

    def _mark(self, tok, reads, writes):
        k = id(tok[0])
        for b in reads:
            b.r[k] = tok
        for b in writes:
            b.w = tok
            b.r = {}

    def op(self, E, fn, reads=(), writes=()):
        self._deps(E, reads, writes)
        if E.count >= self.SEM_ROLL:
            E.sem = self.new_sem(E.name)
            E.count = 0
        ins = fn(E.eng)
        E.count += 1
        ins.then_inc(E.sem, 1)
        self._mark((E.sem, E.count, E), reads, writes)

    def dma(self, Q, out, in_, reads=(), writes=(), **kw):
        self._deps(Q, reads, writes)
        slot = Q.slots[Q.slot_i % self.NSLOT]
        Q.slot_i += 1
        if slot[1] > 0:
            self._wait(Q, (slot[0], slot[1], None))
        if slot[1] >= self.SEM_ROLL:
            slot[0] = self.new_sem(Q.name + "d")
            slot[1] = 0
        slot[1] += 16
        Q.eng.dma_start(out=out, in_=in_, **kw).then_inc(slot[0], 16)
        self._mark((slot[0], slot[1], None), reads, writes)

    def barrier(self):
        toks = [(E.sem, E.count, None) for E in self.engs if E.count > 0]
        for q in (self.sp, self.pool, self.act):
            for s in q.slots:
                if s[1] > 0:
                    toks.append((s[0], s[1], None))
        for E in self.engs:
            for t in toks:
                if t[0] is E.sem:
                    continue
                self._wait(E, t)


def bufs(n, x=False):
    return [Buf(x) for _ in range(n)]


class Consts:
    def __init__(self, nc, S, es):
        self.b = Buf()
        self.idf = es.enter_context(nc.sbuf_tensor("c_idf", [128, 128], F32))
        self.idb = es.enter_context(nc.sbuf_tensor("c_idb", [128, 128], BF16))
        self.mhalf = es.enter_context(nc.sbuf_tensor("c_mhalf", [128, 1], F32))
        S.op(S.pool, lambda e: e.memset(self.idf[:], 1.0), writes=[self.b])
        S.op(S.pool, lambda e: e.affine_select(out=self.idf[:], in_=self.idf[:], pattern=[[-1, 128]],
                                              compare_op=ALU.is_equal, fill=0.0, base=0,
                                              channel_multiplier=1), writes=[self.b])
        S.op(S.pool, lambda e: e.memset(self.mhalf[:], -0.5), writes=[self.b])
        S.op(S.dve, lambda e: e.tensor_copy(out=self.idb[:], in_=self.idf[:]), writes=[self.b])


def rstd_ops(S, C, ss, tmp, out, n, post_scale, rb, wb, dim=D):
    S.op(S.pool, lambda e: e.tensor_scalar(out=tmp, in0=ss, scalar1=1.0 / dim, scalar2=EPS,
                                          op0=ALU.mult, op1=ALU.add), reads=[rb], writes=[wb])
    S.op(S.pool, lambda e: e.tensor_tensor(out=out, in0=tmp, in1=C.mhalf[:].to_broadcast([128, n]),
                                          op=ALU.pow), reads=[C.b], writes=[wb])
    if post_scale != 1.0:
        S.op(S.pool, lambda e: e.tensor_scalar(out=out, in0=out, scalar1=post_scale, scalar2=0.0,
                                              op0=ALU.mult, op1=ALU.add), writes=[wb])


def ffn_phase(nc, S, C, tag, xi, xi_b, xo, xo_b, wg_d, wu_d, wd_d, gpre_d, gpost_d, ntok):
    T = 256
    NT = ntok // T
    with ExitStack() as es:
        def sb(name, shape, dt):
            return es.enter_context(nc.sbuf_tensor(tag + name, shape, dt))

        def ps(name, shape, dt):
            return es.enter_context(nc.psum_tensor(tag + name, shape, dt))

        wg = sb("wg", [128, KC, DFF], BF16)
        wu = sb("wu", [128, KC, DFF], BF16)
        wd = sb("wd", [128, NFF, D], BF16)
        gcol = sb("gcol", [128, KC], F32)
        gpost = sb("gpost", [128, D], F32)
        xin = [sb("xin%d" % i, [128, 2, D], F32) for i in range(2)]
        hT = [sb("hT%d" % i, [128, KC, T], BF16) for i in range(2)]
        xs = [sb("xs%d" % i, [128, D], BF16) for i in range(2)]
        junk = sb("junk", [128, D], BF16)
        sg = [sb("sg%d" % i, [128, T], F32) for i in range(2)]
        actT = [sb("actT%d" % i, [128, T], BF16) for i in range(3)]
        tmp = [sb("tmp%d" % i, [128, D], F32) for i in range(2)]
        st = sb("st", [128, 16], F32)
        ptr = [ps("ptr%d" % i, [128, KC, 128], BF16) for i in range(2)]
        pgu = [ps("pgu%d" % i, [128, 512], F32) for i in range(2)]
        pd = [ps("pd%d" % i, [128, D], F32) for i in range(2)]

        b_wg, b_wu, b_wd, b_g = Buf(), Buf(), Buf(), Buf()
        b_xin = [bufs(2) for _ in range(2)]
        b_hT = [bufs(2) for _ in range(2)]
        b_xs, b_junk, b_sg, b_actT, b_tmp = bufs(2), Buf(), bufs(2), bufs(3), bufs(2)
        b_st = [[Buf() for _ in range(2)] for _ in range(2)]
        b_st2 = bufs(2)
        b_ptr, b_pgu, b_pd = bufs(2, True), bufs(2, True), bufs(2, True)

        wg_v = wg_d.rearrange("(kc p) n -> p kc n", p=128)
        wu_v = wu_d.rearrange("(kc p) n -> p kc n", p=128)
        wd_v = wd_d.rearrange("(fc p) n -> p fc n", p=128)
        for kc in range(KC):
            for h in range(2):
                sl = slice(h * (DFF // 2), (h + 1) * (DFF // 2))
                S.dma(S.pool, wg[:, kc, sl], wg_v[:, kc, sl], writes=[b_wg])
                S.dma(S.pool, wu[:, kc, sl], wu_v[:, kc, sl], writes=[b_wu])
        for fc in range(NFF):
            S.dma(S.pool, wd[:, fc, :], wd_v[:, fc, :], writes=[b_wd])
        with nc.allow_non_contiguous_dma(reason="tiny gain vector transpose load"):
            S.dma(S.sp, gcol[:], gpre_d.rearrange("(kc p) -> p kc", p=128), writes=[b_g])
        S.dma(S.sp, gpost[:], gpost_d.unsqueeze(0).to_broadcast([128, D]), writes=[b_g])

        def load(t):
            par = t % 2
            for s in range(2):
                r0 = t * T + s * 128
                S.dma(S.sp, xin[par][:, s, :], xi[r0:r0 + 128, :],
                      reads=[xi_b[r0 // 128]], writes=[b_xin[par][s]])

        def prologue(t):
            par = t % 2
            for s in range(2):
                q = s
                stt = st[:, (par * 2 + s) * 3:(par * 2 + s) * 3 + 3]
                S.op(S.act, lambda e: e.activation(out=junk[:], in_=xin[par][:, s, :], func=AF.Square,
                                                   accum_out=stt[:, 0:1]),
                     reads=[b_xin[par][s]], writes=[b_junk, b_st[par][s]])
                rstd_ops(S, C, stt[:, 0:1], stt[:, 1:2], stt[:, 2:3], 1, 1.0, b_st[par][s], b_st[par][s])
                S.op(S.dve, lambda e: e.tensor_scalar(out=xs[q][:], in0=xin[par][:, s, :],
                                                      scalar1=stt[:, 2:3], scalar2=None, op0=ALU.mult),
                     reads=[b_xin[par][s], b_st[par][s]], writes=[b_xs[q]])
                for kc in range(KC):
                    S.op(S.pe, lambda e: e.transpose(ptr[q][:, kc, :], xs[q][:, kc * 128:(kc + 1) * 128],
                                                     C.idb[:]),
                         reads=[b_xs[q], C.b], writes=[b_ptr[q]])
                S.op(S.dve, lambda e: e.tensor_tensor(out=hT[par][:, :, s * 128:(s + 1) * 128], in0=ptr[q][:],
                                                      in1=gcol[:].unsqueeze(2).to_broadcast([128, KC, 128]),
                                                      op=ALU.mult),
                     reads=[b_ptr[q], b_g], writes=[b_hT[par][s]])

        def GU(t, fc):
            par = t % 2
            pb = fc % 2
            for (w, bw, off) in ((wg, b_wg, 0), (wu, b_wu, T)):
                for kc in range(KC):
                    S.op(S.pe, lambda e: e.matmul(pgu[pb][:, off:off + T], lhsT=w[:, kc, fc * 128:(fc + 1) * 128],
                                                  rhs=hT[par][:, kc, :], start=(kc == 0), stop=(kc == KC - 1)),
                         reads=[bw, b_hT[par][0], b_hT[par][1]], writes=[b_pgu[pb]])

        def ACTF(t, fc):
            pb = fc % 2
            r = fc % 3
            S.op(S.act, lambda e: e.activation(out=sg[pb][:], in_=pgu[pb][:, 0:T], func=AF.Silu),
                 reads=[b_pgu[pb]], writes=[b_sg[pb]])
            S.op(S.dve, lambda e: e.tensor_tensor(out=actT[r][:], in0=pgu[pb][:, T:2 * T], in1=sg[pb][:],
                                                  op=ALU.mult),
                 reads=[b_pgu[pb], b_sg[pb]], writes=[b_actT[r]])

        def DN(t, fc):
            r = fc % 3
            for s in range(2):
                for h in range(2):
                    S.op(S.pe, lambda e: e.matmul(pd[s][:, h * 512:(h + 1) * 512],
                                                  lhsT=actT[r][:, s * 128:(s + 1) * 128],
                                                  rhs=wd[:, fc, h * 512:(h + 1) * 512],
                                                  start=(fc == 0), stop=(fc == NFF - 1)),
                         reads=[b_actT[r], b_wd], writes=[b_pd[s]])

        def epilogue(t):
            par = t % 2
            for s in range(2):
                stt = st[:, 12 + s * 2:12 + s * 2 + 2]
                S.op(S.act, lambda e: e.activation(out=junk[:], in_=pd[s][:], func=AF.Square,
                                                   accum_out=stt[:, 0:1]),
                     reads=[b_pd[s]], writes=[b_junk, b_st2[s]])
                rstd_ops(S, C, stt[:, 0:1], stt[:, 1:2], stt[:, 1:2], 1, 0.5, b_st2[s], b_st2[s])
                S.op(S.dve, lambda e: e.scalar_tensor_tensor(out=tmp[s][:], in0=pd[s][:], scalar=stt[:, 1:2],
                                                             in1=gpost[:], op0=ALU.mult, op1=ALU.mult),
                     reads=[b_pd[s], b_st2[s], b_g], writes=[b_tmp[s]])
                S.op(S.pool, lambda e: e.tensor_tensor(out=xin[par][:, s, :], in0=xin[par][:, s, :],
                                                       in1=tmp[s][:], op=ALU.add),
                     reads=[b_tmp[s]], writes=[b_xin[par][s]])
                r0 = t * T + s * 128
                S.dma(S.sp, xo[r0:r0 + 128, :], xin[par][:, s, :],
                      reads=[b_xin[par][s]], writes=[xo_b[r0 // 128]])

        load(0)
        prologue(0)
        for t in range(NT):
            if t + 1 < NT:
                load(t + 1)
            GU(t, 0)
            for fc in range(NFF):
                if fc + 1 < NFF:
                    GU(t, fc + 1)
                if fc == 8 and t + 1 < NT:
                    prologue(t + 1)
                ACTF(t, fc)
                DN(t, fc)
            epilogue(t)
        S.barrier()


O_AQ, O_AK, O_AV, O_IQ, O_IK, O_IW, O_GQ, O_GK, O_GV, O_GA, O_GG = (
    0, 512, 640, 768, 1280, 1344, 1352, 1608, 1864, 2376, 2392)
NIN = 2904
NIT = 16
SCHED = {10: 1, 11: 2, 12: 3, 13: 4, 14: 5, 15: 6}
BIG = 240000.0
IWS = (8.0 ** -0.5) * (64.0 ** -0.5)


def mix_phase(nc, S, C, xi, xi_b, xo, xo_b, W, ntok):
    NT = ntok // 128
    TOPK = min(256, ntok // 4)
    SC_W = ntok
    CMB_W = max(ntok, 8192)
    with ExitStack() as es:
        def sb(name, shape, dt):
            return es.enter_context(nc.sbuf_tensor("mx" + name, shape, dt))

        def ps(name, shape, dt):
            return es.enter_context(nc.psum_tensor("mx" + name, shape, dt))

        win = sb("win", [128, KC, NIN], BF16)
        cmb = sb("cmb", [128, CMB_W], BF16)
        woa = cmb[0:64, 0:8192].rearrange("p (h n) -> p h n", h=8)
        wog = sb("wog", [128, 4, D], BF16)
        wa2 = sb("wa2", [16, 256], BF16)
        KT = sb("KT", [128, ntok], BF16)
        Vaug = sb("Vaug", [128, NT, 2, 65], BF16)
        sc = sb("sc", [128, SC_W], F32)
        MB = sb("MB", [128, ntok], BF16)
        gcol = sb("gcol", [128, KC], F32)
        gpost = sb("gpost", [128, D], F32)
        gnorm = sb("gnorm", [128, 128], F32)
        babc = sb("babc", [128, 256], F32)
        M1s = sb("M1s", [128, 16], F32)
        Gsel = sb("Gsel", [128, 8], F32)
        I4 = sb("I4", [128, 4, 128], BF16)
        TriU = sb("TriU", [128, 128], F32)
        TriL2 = sb("TriL2", [128, 128], F32)
        CM = sb("CM", [128, 128], F32)
        pow2 = sb("pow2", [128, NIT + 1], F32)
        ones32 = sb("ones32", [128, 64], F32)
        thr_all = sb("thrall", [128, 1], F32)
        xt = sb("xt", [128, D], F32)
        xs = sb("xs", [128, D], BF16)
        hT = sb("hT", [128, KC, 128], BF16)
        QTs = [sb("QT%d" % k, [128, 8, 128], BF16) for k in range(2)]
        IQT = sb("IQT", [128, 8, 8, 16], BF16)
        IQTv = IQT[64:128, :, :, :]
        OTt = sb("OTt", [64, 1024], BF16)
        gqT = sb("gqT", [128, 256], F32)
        gkT = sb("gkT", [128, 256], F32)
        gaT = sb("gaT", [16, 128], BF16)
        iwtok = sb("iwtok", [128, 8], F32)
        gktok = sb("gktok", [128, 256], F32)
        gvbf = sb("gvbf", [128, 512], BF16)
        ggs = xt[:, 512:1024]
        zs = sb("zs", [128, 256], F32)
        E1 = sb("E1", [128, 2, 128], F32)
        E2 = sb("E2", [128, 256], F32)
        E3 = sb("E3", [128, 256], F32)
        qd = sb("qd", [128, 2, 128], BF16)
        kin = sb("kin", [128, 2, 128], BF16)
        kout = sb("kout", [128, 256], BF16)
        AT = [sb("AT%d" % k, [128, 128], BF16) for k in range(2)]
        Sst = sb("Sst", [128, 2, 128], F32)
        S0 = sb("S0", [128, 2, 128], BF16)
        S1 = sb("S1", [128, 2, 128], BF16)
        junkg = sb("junkg", [128, 128], BF16)
        ogla = sb("ogla", [128, 4, 128], BF16)
        ogTs = [sb("ogT%d" % k, [128, 4, 128], BF16) for k in range(2)]
        nrm = xt[:, 0:512]
        eg = nrm
        cj = sb("cj", [128, ntok], mybir.dt.uint8)
        Wsel = sb("Wsel", [128, 8, 128], BF16)
        L = sb("L", [128, 8, 16], F32)
        iwcol = sb("iwcol", [128, 8], F32)
        R = [sb("R%d" % k, [128, 512], BF16) for k in range(3)]
        NPT = 2
        PT = [sb("PT%d" % k, [128, 512], BF16) for k in range(NPT)]
        OT = [OTt[:, k * 512:(k + 1) * 512] for k in range(2)]
        st = sb("st", [128, 16], F32)
        bs = sb("bs", [128, 4], F32)
        hst = sb("hst", [128, NIT + 1], F32)
        mid = sb("mid", [128, 1], F32)
        cnt = sb("cnt", [128, 1], F32)
        gp = sb("gp", [128, 1], F32)
        thr = sb("thr", [128, 1], F32)

        ptr = ps("ptr", [128, KC, 128], BF16)
        pool = [ps("pb%d" % k, [128, 512], F32) for k in range(5)]
        acc = ps("acc", [128, 1024], F32)

        b_pool = bufs(5, True)
        b_acc = bufs(2, True)
        b_ptr, b_win, b_woa, b_wog, b_c = Buf(True), Buf(), Buf(), Buf(), Buf()
        b_KT, b_IKT, b_V = bufs(NT), bufs(NT), bufs(NT)
        b_sc, b_MB = Buf(), Buf()
        b_xts, b_QTs, b_ogTs, b_nrm, b_cj, b_Wsel = bufs(2), bufs(2), bufs(2), Buf(), Buf(), Buf()
        (b_xt, b_xs, b_hT, b_QT, b_IQT, b_gqT, b_gkT, b_gaT, b_iw, b_gktok, b_gv, b_nrm, b_gg, b_zs, b_E1,
         b_E2, b_E3, b_qd, b_kin, b_kout, b_Sst, b_S0, b_S1, b_junkg, b_ogla, b_ogT, b_L, b_iwcol, b_st,
         b_stg, b_bs, b_hst, b_mid, b_cnt, b_gp, b_thr) = bufs(36)
        b_AT, b_R, b_PT, b_OT = bufs(2), bufs(3), bufs(NPT), bufs(2)
        b_nrm = b_xt
        b_gg = b_xt

        rc = {"a": 0, "i": 0, "p": 0}

        def rot(key, lo, n):
            k = lo + rc[key] % n
            rc[key] += 1
            return pool[k], b_pool[k]

        win_v = W["w_in"].rearrange("(kc p) n -> p kc n", p=128)
        hw = NIN // 2
        for kc in range(KC):
            for h in range(2):
                S.dma(S.pool, win[:, kc, h * hw:(h + 1) * hw], win_v[:, kc, h * hw:(h + 1) * hw], writes=[b_win])
        for h in range(8):
            S.dma(S.pool, cmb[0:64, h * 1024:(h + 1) * 1024], W["w_out"][h * 64:(h + 1) * 64, :], writes=[b_woa])
        for hg in range(4):
            S.dma(S.pool, wog[:, hg, :], W["w_out"][512 + hg * 128:512 + (hg + 1) * 128, :], writes=[b_wog])
        S.dma(S.pool, wa2[:], W["w_gla_a2"], writes=[b_c])
        with nc.allow_non_contiguous_dma(reason="tiny gain vector transpose load"):
            S.dma(S.sp, gcol[:], W["g_mix_pre"].rearrange("(kc p) -> p kc", p=128), writes=[b_c])
        S.dma(S.sp, gpost[:], W["g_mix_post"].unsqueeze(0).to_broadcast([128, D]), writes=[b_c])
        S.dma(S.sp, gnorm[:], W["g_gla_norm"].unsqueeze(0).to_broadcast([128, 128]), writes=[b_c])
        S.dma(S.sp, babc[:], W["b_gla_a"].unsqueeze(0).to_broadcast([128, 256]), writes=[b_c])

        P = S.pool
        S.op(P, lambda e: e.memset(Vaug[:], 1.0), writes=b_V)
        S.op(P, lambda e: e.memset(Sst[:], 0.0), writes=[b_Sst])
        S.op(P, lambda e: e.memset(S0[:], 0.0), writes=[b_S0])
        S.op(P, lambda e: e.memset(thr_all[:], -1e29), writes=[b_c])
        for k in range(2):
            S.op(P, lambda e: e.memset(QTs[k][:], 0.0), writes=[b_QTs[k]])
        S.op(P, lambda e: e.memset(IQT[:], 0.0), writes=[b_IQT])
        S.op(P, lambda e: e.memset(ones32[:], 1.0), writes=[b_c])
        S.op(P, lambda e: e.memset(Wsel[:], 0.0), writes=[b_Wsel])
        for k in range(NIT + 1):
            S.op(P, lambda e: e.memset(pow2[:, k:k + 1], 2.0 ** -(k + 1)), writes=[b_c])
        S.op(P, lambda e: e.memset(TriU[:], 1.0), writes=[b_c])
        S.op(P, lambda e: e.affine_select(out=TriU[:], in_=TriU[:], pattern=[[1, 128]], compare_op=ALU.is_ge,
                                          fill=0.0, base=0, channel_multiplier=-1), writes=[b_c])
        S.op(P, lambda e: e.memset(TriU[0:64, 64:128], 0.0), writes=[b_c])
        S.op(P, lambda e: e.memset(TriL2[:], 1.0), writes=[b_c])
        S.op(P, lambda e: e.affine_select(out=TriL2[:], in_=TriL2[:], pattern=[[-1, 128]], compare_op=ALU.is_ge,
                                          fill=0.0, base=-1, channel_multiplier=1), writes=[b_c])
        S.op(P, lambda e: e.memset(TriL2[64:128, 0:64], 0.0), writes=[b_c])
        S.op(P, lambda e: e.memset(CM[:], 0.0), writes=[b_c])
        S.op(P, lambda e: e.affine_select(out=CM[:], in_=CM[:], pattern=[[-1, 128]], compare_op=ALU.is_ge,
                                          fill=-1e30, base=0, channel_multiplier=1), writes=[b_c])
        V_ = S.dve
        S.op(V_, lambda e: e.tensor_reduce(out=M1s[:], in_=C.idf[:].rearrange("p (j t) -> p t j", t=16),
                                           axis=AX.X, op=ALU.add), reads=[C.b], writes=[b_c])
        S.op(V_, lambda e: e.tensor_reduce(out=Gsel[:], in_=C.idf[:].rearrange("p (j t) -> p j t", t=16),
                                           axis=AX.X, op=ALU.add), reads=[C.b], writes=[b_c])
        S.op(V_, lambda e: e.tensor_copy(out=I4[:], in_=C.idb[:].unsqueeze(1).to_broadcast([128, 4, 128])),
             reads=[C.b], writes=[b_c])

        def fm(bank, bb, slot, c0, M, pbase=0):
            for kc in range(KC):
                S.op(S.pe, lambda e: e.matmul(bank[pbase:pbase + M, slot * 128:(slot + 1) * 128],
                                              lhsT=win[:, kc, c0:c0 + M], rhs=hT[:, kc, :],
                                              start=(kc == 0), stop=(kc == KC - 1)),
                     reads=[b_win, b_hT], writes=[bb])

        def tm(bank, bb, o0, c0, N):
            for kc in range(KC):
                S.op(S.pe, lambda e: e.matmul(bank[:, o0:o0 + N], lhsT=hT[:, kc, :], rhs=win[:, kc, c0:c0 + N],
                                              start=(kc == 0), stop=(kc == KC - 1)),
                     reads=[b_win, b_hT], writes=[bb])

        def tile_A(i):
            r0 = i * 128
            QT, b_QT = QTs[i % 2], b_QTs[i % 2]
            S.dma(S.sp, xt[:], xi[r0:r0 + 128, :], reads=[xi_b[i]], writes=[b_xt])
            S.op(S.act, lambda e: e.activation(out=xs[:], in_=xt[:], func=AF.Square, accum_out=st[:, 0:1]),
                 reads=[b_xt], writes=[b_xs, b_st])
            rstd_ops(S, C, st[:, 0:1], st[:, 1:2], st[:, 2:3], 1, 1.0, b_st, b_st)
            S.op(S.dve, lambda e: e.tensor_scalar(out=xs[:], in0=xt[:], scalar1=st[:, 2:3], scalar2=None,
                                                  op0=ALU.mult), reads=[b_xt, b_st], writes=[b_xs])
            for kc in range(KC):
                S.op(S.pe, lambda e: e.transpose(ptr[:, kc, :], xs[:, kc * 128:(kc + 1) * 128], C.idb[:]),
                     reads=[b_xs, C.b], writes=[b_ptr])
            S.op(S.dve, lambda e: e.tensor_tensor(out=hT[:], in0=ptr[:],
                                                  in1=gcol[:].unsqueeze(2).to_broadcast([128, KC, 128]),
                                                  op=ALU.mult), reads=[b_ptr, b_c], writes=[b_hT])
            yield
            bk, bb = rot("a", 0, 5)
            for r in range(4):
                fm(bk, bb, r, O_AQ + r * 64, 64, 0)
                fm(bk, bb, r, O_AQ + (r + 4) * 64, 64, 64)
            S.op(S.act, lambda e: e.copy(out=QT[0:64, 0:4, :].rearrange("p r t -> p (r t)"), in_=bk[0:64, :]),
                 reads=[bb], writes=[b_QT])
            S.op(S.act, lambda e: e.copy(out=QT[64:128, 4:8, :].rearrange("p r t -> p (r t)"), in_=bk[64:128, :]),
                 reads=[bb], writes=[b_QT])
            for hb in range(2):
                yield
                bk, bb = rot("a", 0, 5)
                for hh in range(4):
                    fm(bk, bb, hh, O_IQ + (hb * 4 + hh) * 64, 64, 64)
                for hh in range(4):
                    S.op(S.act, lambda e: e.copy(out=IQTv[:, :, hb * 4 + hh, :],
                                                 in_=bk[64:128, hh * 128:(hh + 1) * 128].rearrange(
                                                     "p (j t) -> p j t", t=16)),
                         reads=[bb], writes=[b_IQT])
            yield
            bk, bb = rot("a", 0, 5)
            fm(bk, bb, 0, O_AK, 128)
            fm(bk, bb, 1, O_IK, 64, 64)
            fm(bk, bb, 2, O_GQ, 128)
            fm(bk, bb, 3, O_GQ + 128, 128)
            S.op(S.dve, lambda e: e.tensor_copy(out=KT[:, r0:r0 + 128], in_=bk[:, 0:128]),
                 reads=[bb], writes=[b_KT[i]])
            S.op(S.dve, lambda e: e.tensor_copy(out=cmb[64:128, r0:r0 + 128], in_=bk[64:128, 128:256]),
                 reads=[bb], writes=[b_IKT[i]])
            S.op(S.act, lambda e: e.copy(out=gqT[:], in_=bk[:, 256:512]), reads=[bb], writes=[b_gqT])
            yield
            bk, bb = rot("a", 0, 5)
            fm(bk, bb, 0, O_GK, 128)
            fm(bk, bb, 1, O_GK + 128, 128)
            fm(bk, bb, 2, O_GA, 16)
            S.op(S.act, lambda e: e.copy(out=gkT[:], in_=bk[:, 0:256]), reads=[bb], writes=[b_gkT])
            S.op(S.dve, lambda e: e.tensor_copy(out=gaT[:], in_=bk[0:16, 256:384]), reads=[bb], writes=[b_gaT])
            yield
            bk, bb = rot("a", 0, 5)
            tm(bk, bb, 0, O_AV, 128)
            tm(bk, bb, 128, O_IW, 8)
            tm(bk, bb, 136, O_GK, 256)
            S.op(S.dve, lambda e: e.tensor_copy(out=Vaug[:, i, :, 0:64],
                                                in_=bk[:, 0:128].rearrange("p (g d) -> p g d", g=2)),
                 reads=[bb], writes=[b_V[i]])
            S.op(S.dve, lambda e: e.tensor_scalar(out=iwtok[:], in0=bk[:, 128:136], scalar1=IWS, scalar2=None,
                                                  op0=ALU.mult), reads=[bb], writes=[b_iw])
            S.op(S.act, lambda e: e.copy(out=gktok[:], in_=bk[:, 136:392]), reads=[bb], writes=[b_gktok])
            yield
            bk, bb = rot("a", 0, 5)
            tm(bk, bb, 0, O_GV, 512)
            S.op(S.dve, lambda e: e.tensor_copy(out=gvbf[:], in_=bk[:, :]), reads=[bb], writes=[b_gv])
            yield
            bk, bb = rot("a", 0, 5)
            tm(bk, bb, 0, O_GG, 512)
            S.op(S.act, lambda e: e.activation(out=eg[:], in_=bk[:, :], func=AF.Exp, scale=-1.0),
                 reads=[bb], writes=[b_nrm])
            S.op(S.dve, lambda e: e.tensor_copy(out=ggs[:], in_=bk[:, :]), reads=[bb], writes=[b_gg])
            S.op(S.act, lambda e: e.activation(out=eg[:], in_=eg[:], func=AF.Ln, bias=1.0), writes=[b_nrm])
            S.op(S.act, lambda e: e.activation(out=eg[:], in_=eg[:], func=AF.Exp, scale=-1.0), writes=[b_nrm])
            S.op(S.pool, lambda e: e.tensor_tensor(out=ggs[:], in0=ggs[:], in1=eg[:], op=ALU.mult),
                 reads=[b_nrm], writes=[b_gg])
            S.op(S.pool, lambda e: e.tensor_tensor(out=ggs[:].rearrange("p (h v) -> p h v", h=4),
                                                   in0=ggs[:].rearrange("p (h v) -> p h v", h=4),
                                                   in1=gnorm[:].unsqueeze(1).to_broadcast([128, 4, 128]),
                                                   op=ALU.mult), reads=[b_c], writes=[b_gg])

        def oreg(h):
            f, a = h // 2, h % 2
            return acc[:, a * 512 + f * 128:a * 512 + (f + 1) * 128]

        def tile_B(i):
            ogT, b_ogT = ogTs[i % 2], b_ogTs[i % 2]
            bk, bb = rot("a", 0, 5)
            S.op(S.pe, lambda e: e.matmul(bk[:, 0:256], lhsT=gaT[:], rhs=wa2[:], start=True, stop=True),
                 reads=[b_gaT, b_c], writes=[bb])
            S.op(S.dve, lambda e: e.tensor_tensor(out=zs[:], in0=bk[:, 0:256], in1=babc[:], op=ALU.add),
                 reads=[bb, b_c], writes=[b_zs])
            S.op(S.act, lambda e: e.activation(out=zs[:], in_=zs[:], func=AF.Exp, scale=-1.0), writes=[b_zs])
            S.op(S.act, lambda e: e.activation(out=zs[:], in_=zs[:], func=AF.Ln, bias=1.0), writes=[b_zs])
            yield
            cb, cbb = rot("a", 0, 5)
            for f in range(2):
                S.op(S.pe, lambda e: e.matmul(cb[:, f * 128:(f + 1) * 128], lhsT=zs[:, f * 128:(f + 1) * 128],
                                              rhs=TriU[:], start=True, stop=True),
                     reads=[b_zs, b_c], writes=[cbb])
            S.op(S.pe, lambda e: e.matmul(cb[:, 256:512], lhsT=TriL2[:], rhs=zs[:], start=True, stop=True),
                 reads=[b_zs, b_c], writes=[cbb])
            S.op(S.act, lambda e: e.activation(out=E1[:].rearrange("p f t -> p (f t)"), in_=cb[:, 0:256],
                                               func=AF.Exp, scale=-1.0 / 16), reads=[cbb], writes=[b_E1])
            S.op(S.act, lambda e: e.activation(out=E2[:], in_=cb[:, 0:256], func=AF.Exp, scale=1.0 / 16),
                 reads=[cbb], writes=[b_E2])
            S.op(S.act, lambda e: e.activation(out=E3[:], in_=cb[:, 256:512], func=AF.Exp, scale=-1.0 / 16),
                 reads=[cbb], writes=[b_E3])
            yield
            S.op(S.dve, lambda e: e.scalar_tensor_tensor(out=qd[:].rearrange("p f t -> p (f t)"), in0=gqT[:],
                                                         scalar=0.125, in1=E1[:].rearrange("p f t -> p (f t)"),
                                                         op0=ALU.mult, op1=ALU.mult),
                 reads=[b_gqT, b_E1], writes=[b_qd])
            S.op(S.dve, lambda e: e.tensor_tensor(out=kin[:].rearrange("p f t -> p (f t)"), in0=gkT[:], in1=E2[:],
                                                  op=ALU.mult), reads=[b_gkT, b_E2], writes=[b_kin])
            S.op(S.dve, lambda e: e.tensor_tensor(out=kout[:], in0=gktok[:], in1=E3[:], op=ALU.mult),
                 reads=[b_gktok, b_E3], writes=[b_kout])

            def state_step(c, Sdst, b_Sdst):
                ub, ubb = rot("a", 0, 5)
                for h in range(4):
                    f, a = h // 2, h % 2
                    S.op(S.pe, lambda e: e.matmul(ub[a * 64:(a + 1) * 64, f * 128:(f + 1) * 128],
                                                  lhsT=kout[c * 64:(c + 1) * 64, h * 64:(h + 1) * 64],
                                                  rhs=gvbf[c * 64:(c + 1) * 64, h * 128:(h + 1) * 128],
                                                  start=True, stop=True),
                         reads=[b_kout, b_gv], writes=[ubb])
                for f in range(2):
                    S.op(S.dve, lambda e: e.scalar_tensor_tensor(out=Sst[:, f, :], in0=Sst[:, f, :],
                                                                 scalar=E1[:, f, c * 64 + 63:c * 64 + 64],
                                                                 in1=ub[:, f * 128:(f + 1) * 128],
                                                                 op0=ALU.mult, op1=ALU.add),
                         reads=[ubb, b_E1], writes=[b_Sst])
                S.op(S.act, lambda e: e.copy(out=Sdst[:].rearrange("p f v -> p (f v)"),
                                             in_=Sst[:].rearrange("p f v -> p (f v)")),
                     reads=[b_Sst], writes=[b_Sdst])

            yield
            state_step(0, S1, b_S1)
            for h in range(4):
                yield
                f, a = h // 2, h % 2
                rows = slice(a * 64, (a + 1) * 64)
                ab, abb = rot("a", 0, 5)
                S.op(S.pe, lambda e: e.matmul(ab[:, 0:128], lhsT=kin[rows, f, :], rhs=qd[rows, f, :],
                                              start=True, stop=True), reads=[b_kin, b_qd], writes=[abb])
                S.op(S.dve, lambda e: e.tensor_tensor(out=AT[h % 2][:], in0=ab[:, 0:128], in1=TriU[:], op=ALU.mult),
                     reads=[abb, b_c], writes=[b_AT[h % 2]])
                o_ = oreg(h)
                S.op(S.pe, lambda e: e.matmul(o_, lhsT=AT[h % 2][:], rhs=gvbf[:, h * 128:(h + 1) * 128],
                                              start=True, stop=False),
                     reads=[b_AT[h % 2], b_gv], writes=[b_acc[a]])
                S.op(S.pe, lambda e: e.matmul(o_[0:64, :], lhsT=qd[rows, f, 0:64], rhs=S0[rows, f, :],
                                              start=False, stop=True),
                     reads=[b_qd, b_S0], writes=[b_acc[a]])
                S.op(S.pe, lambda e: e.matmul(o_[64:128, :], lhsT=qd[rows, f, 64:128], rhs=S1[rows, f, :],
                                              start=False, stop=True),
                     reads=[b_qd, b_S1], writes=[b_acc[a]])
            yield
            state_step(1, S0, b_S0)
            yield
            for h in range(4):
                S.op(S.act, lambda e: e.activation(out=junkg[:], in_=oreg(h), func=AF.Square,
                                                   accum_out=st[:, 4 + h:5 + h]),
                     reads=[b_acc[h % 2]], writes=[b_junkg, b_stg])
            rstd_ops(S, C, st[:, 4:8], st[:, 8:12], st[:, 12:16], 4, 1.0, b_stg, b_stg, dim=128)
            yield
            for h in range(4):
                S.op(S.dve, lambda e: e.scalar_tensor_tensor(out=ogla[:, h, :], in0=oreg(h),
                                                             scalar=st[:, 12 + h:13 + h],
                                                             in1=ggs[:, h * 128:(h + 1) * 128],
                                                             op0=ALU.mult, op1=ALU.mult),
                     reads=[b_acc[h % 2], b_stg, b_gg], writes=[b_ogla])
            yield
            for h in range(4):
                S.op(S.pe, lambda e: e.transpose(ptr[:, h, :], ogla[:, h, :], C.idb[:]),
                     reads=[b_ogla, C.b], writes=[b_ptr])
            S.op(S.act, lambda e: e.copy(out=ogT[:], in_=ptr[:, 0:4, :]), reads=[b_ptr], writes=[b_ogT])

        def tile_C(i):
            NK = 128 * (i + 1)
            S.op(S.dve, lambda e: e.tensor_tensor(out=L[:], in0=iwtok[:].unsqueeze(2).to_broadcast([128, 8, 16]),
                                                  in1=M1s[:].unsqueeze(1).to_broadcast([128, 8, 16]),
                                                  op=ALU.mult), reads=[b_iw, b_c], writes=[b_L])
            wb_, wbb = pool[4], b_pool[4]
            S.op(S.pe, lambda e: e.matmul(wb_[:, 0:8], lhsT=L[:].rearrange("p h t -> p (h t)"), rhs=Gsel[:],
                                          start=True, stop=True), reads=[b_L, b_c], writes=[wbb])
            S.op(S.act, lambda e: e.copy(out=iwcol[:], in_=wb_[:, 0:8]), reads=[wbb], writes=[b_iwcol])
            for j in range(8):
                S.op(S.dve, lambda e: e.tensor_scalar(out=Wsel[:, j, 16 * j:16 * j + 16], in0=M1s[:],
                                                      scalar1=iwcol[:, j:j + 1], scalar2=None, op0=ALU.mult),
                     reads=[b_iwcol, b_c], writes=[b_Wsel])
            nkb = (NK + 511) // 512
            for kb in range(nkb):
                wdt = min(512, NK - kb * 512)
                kts = [b_IKT[t] for t in range(kb * 4, kb * 4 + wdt // 128)]
                sb_, sbb = pool[3 + kb % 2], b_pool[3 + kb % 2]

                def LG(j):
                    lb, lbb = rot("i", 0, 3)
                    S.op(S.pe, lambda e: e.matmul(lb[:, 0:wdt],
                                                  lhsT=IQT[:, j, :, :].rearrange("p h t -> p (h t)"),
                                                  rhs=cmb[:, kb * 512:kb * 512 + wdt],
                                                  start=True, stop=True), reads=[b_IQT, b_woa] + kts, writes=[lbb])
                    return lb, lbb

                cur = LG(0)
                for j in range(8):
                    nxt = LG(j + 1) if j + 1 < 8 else None
                    lb, lbb = cur
                    r = j % 3
                    if j % 2 == 0:
                        S.op(S.act, lambda e: e.activation(out=R[r][:, 0:wdt], in_=lb[:, 0:wdt], func=AF.Relu),
                             reads=[lbb], writes=[b_R[r]])
                    else:
                        S.op(S.dve, lambda e: e.tensor_scalar(out=R[r][:, 0:wdt], in0=lb[:, 0:wdt], scalar1=0.0,
                                                              scalar2=None, op0=ALU.max),
                             reads=[lbb], writes=[b_R[r]])
                    S.op(S.pe, lambda e: e.matmul(sb_[:, 0:wdt], lhsT=Wsel[:, j, :], rhs=R[r][:, 0:wdt],
                                                  start=(j == 0), stop=(j == 7)),
                         reads=[b_R[r], b_Wsel], writes=[sbb])
                    cur = nxt
                S.op(S.dve, lambda e: e.tensor_copy(out=sc[:, kb * 512:kb * 512 + wdt], in_=sb_[:, 0:wdt]),
                     reads=[sbb], writes=[b_sc])
            bis = NK > TOPK
            if bis:
                S.op(S.dve, lambda e: e.tensor_reduce(out=bs[:, 0:1], in_=sc[:, 0:NK], axis=AX.X, op=ALU.max),
                     reads=[b_sc], writes=[b_bs])
                S.op(S.dve, lambda e: e.tensor_reduce(out=bs[:, 1:2], in_=sc[:, 0:NK], axis=AX.X, op=ALU.min),
                     reads=[b_sc], writes=[b_bs])
                S.op(S.dve, lambda e: e.tensor_tensor(out=bs[:, 2:3], in0=bs[:, 0:1], in1=bs[:, 1:2],
                                                      op=ALU.subtract), writes=[b_bs])
                S.op(S.dve, lambda e: e.tensor_scalar(out=hst[:], in0=pow2[:], scalar1=bs[:, 2:3], scalar2=None,
                                                      op0=ALU.mult), reads=[b_bs, b_c], writes=[b_hst])
                S.op(S.dve, lambda e: e.tensor_tensor(out=mid[:], in0=bs[:, 1:2], in1=hst[:, 0:1], op=ALU.add),
                     reads=[b_bs, b_hst], writes=[b_mid])
            S.op(S.dve, lambda e: e.tensor_tensor(out=sc[:, NK - 128:NK], in0=sc[:, NK - 128:NK], in1=CM[:],
                                                  op=ALU.add), reads=[b_c], writes=[b_sc])

        def bisect(i, g):
            NK = 128 * (i + 1)
            if i >= 1:
                next(g, None)
            for k in range(NIT):
                S.op(S.dve, lambda e: e.tensor_scalar(out=cj[:, 0:NK], in0=sc[:, 0:NK], scalar1=mid[:, 0:1],
                                                      scalar2=None, op0=ALU.is_ge, op1=ALU.add,
                                                      accum_out=cnt[:, 0:1]),
                     reads=[b_sc, b_mid], writes=[b_cj, b_cnt])
                S.op(S.dve, lambda e: e.tensor_scalar(out=gp[:], in0=cnt[:], scalar1=float(TOPK),
                                                      scalar2=hst[:, k:k + 1], op0=ALU.is_ge, op1=ALU.mult),
                     reads=[b_cnt, b_hst], writes=[b_gp])
                S.op(S.dve, lambda e: e.scalar_tensor_tensor(out=mid[:], in0=mid[:],
                                                             scalar=hst[:, k + 1:k + 2], in1=gp[:],
                                                             op0=ALU.subtract, op1=ALU.add),
                     reads=[b_gp, b_hst], writes=[b_mid])
                for _ in range(SCHED.get(k, 0)):
                    next(g, None)
            S.op(S.dve, lambda e: e.tensor_tensor(out=thr[:], in0=mid[:], in1=hst[:, NIT:NIT + 1],
                                                  op=ALU.subtract), reads=[b_mid, b_hst], writes=[b_thr])

        def tile_C2(i):
            NK = 128 * (i + 1)
            tap = thr if NK > TOPK else thr_all
            S.op(S.dve, lambda e: e.tensor_scalar(out=MB[:, 0:NK], in0=sc[:, 0:NK], scalar1=tap[:, 0:1],
                                                  scalar2=-BIG, op0=ALU.is_lt, op1=ALU.mult),
                 reads=[b_sc, b_thr, b_c], writes=[b_MB])

        def tile_D(i):
            QT, b_QT = QTs[i % 2], b_QTs[i % 2]
            QT2 = QT[:].rearrange("p h t -> p (h t)")
            I42 = I4[:].rearrange("p r t -> p (r t)")

            def QK(kb2):
                res = []
                for g in range(2):
                    pb_, pbb = rot("p", 0, 4)
                    S.op(S.pe, lambda e: e.matmul(pb_[:, :], lhsT=KT[:, kb2 * 128:(kb2 + 1) * 128],
                                                  rhs=QT2[:, g * 512:(g + 1) * 512], start=True, stop=False),
                         reads=[b_KT[kb2], b_QT], writes=[pbb])
                    S.op(S.pe, lambda e: e.matmul(pb_[:, :], lhsT=MB[:, kb2 * 128:(kb2 + 1) * 128], rhs=I42,
                                                  start=False, stop=True), reads=[b_MB, b_c], writes=[pbb])
                    res.append((pb_, pbb))
                return res

            cur = QK(0)
            for kb2 in range(i + 1):
                nxt = QK(kb2 + 1) if kb2 < i else None
                for g in range(2):
                    pb_, pbb = cur[g]
                    r = (2 * kb2 + g) % NPT
                    S.op(S.act, lambda e: e.activation(out=PT[r][:], in_=pb_[:, :], func=AF.Exp, scale=0.125),
                         reads=[pbb], writes=[b_PT[r]])
                    S.op(S.pe, lambda e: e.matmul(acc[0:65, g * 512:(g + 1) * 512], lhsT=Vaug[:, kb2, g, :],
                                                  rhs=PT[r][:], start=(kb2 == 0), stop=(kb2 == i)),
                         reads=[b_V[kb2], b_PT[r]], writes=[b_acc[g]])
                cur = nxt
            p4, b_p4 = pool[4], b_pool[4]
            for g in range(2):
                yield
                rrow = nrm[64:65, :]
                S.op(S.act, lambda e: e.activation(out=rrow, in_=acc[64:65, g * 512:(g + 1) * 512], func=AF.Ln),
                     reads=[b_acc[g]], writes=[b_nrm])
                S.op(S.act, lambda e: e.activation(out=rrow, in_=rrow, func=AF.Exp, scale=-1.0), writes=[b_nrm])
                S.op(S.pe, lambda e: e.matmul(p4[0:64, :], lhsT=ones32[64:65, 0:64], rhs=rrow,
                                              start=True, stop=True), reads=[b_nrm, b_c], writes=[b_p4])
                S.op(S.act, lambda e: e.copy(out=nrm[0:64, :], in_=p4[0:64, :]),
                     reads=[b_p4], writes=[b_nrm])
                S.op(S.dve, lambda e: e.tensor_tensor(out=OT[g], in0=acc[0:64, g * 512:(g + 1) * 512],
                                                      in1=nrm[0:64, :], op=ALU.mult),
                     reads=[b_acc[g], b_nrm], writes=[b_OT[g]])

        def tile_E(i):
            r0 = i * 128
            ogT, b_ogT = ogTs[i % 2], b_ogTs[i % 2]
            for half in range(2):
                cols = slice(half * 512, (half + 1) * 512)
                for h in range(8):
                    g, r = h // 4, h % 4
                    S.op(S.pe, lambda e: e.matmul(acc[:, cols], lhsT=OT[g][:, r * 128:(r + 1) * 128],
                                                  rhs=woa[:, h, cols], start=(h == 0), stop=False),
                         reads=[b_OT[g], b_woa], writes=[b_acc[half]])
                for hg in range(4):
                    S.op(S.pe, lambda e: e.matmul(acc[:, cols], lhsT=ogT[:, hg, :], rhs=wog[:, hg, cols],
                                                  start=False, stop=(hg == 3)),
                         reads=[b_ogT, b_wog], writes=[b_acc[half]])
            yield
            S.op(S.act, lambda e: e.activation(out=xs[:], in_=acc[:, :], func=AF.Square, accum_out=st[:, 0:1]),
                 reads=[b_acc[0], b_acc[1]], writes=[b_xs, b_st])
            rstd_ops(S, C, st[:, 0:1], st[:, 1:2], st[:, 2:3], 1, 1.0, b_st, b_st)
            S.op(S.dve, lambda e: e.scalar_tensor_tensor(out=acc[:, :], in0=acc[:, :], scalar=st[:, 2:3],
                                                         in1=gpost[:], op0=ALU.mult, op1=ALU.mult),
                 reads=[b_st, b_c], writes=[b_acc[0], b_acc[1]])
            for half in range(2):
                yield
                cols = slice(half * 512, (half + 1) * 512)
                S.dma(S.sp, nrm[:], xi[r0:r0 + 128, cols], reads=[xi_b[i]], writes=[b_nrm])
                S.op(S.dve, lambda e: e.tensor_tensor(out=nrm[:], in0=acc[:, cols], in1=nrm[:], op=ALU.add),
                     reads=[b_acc[half]], writes=[b_nrm])
                S.dma(S.sp, xo[r0:r0 + 128, cols], nrm[:], reads=[b_nrm], writes=[xo_b[i]])

        def run_all(g):
            for _ in g:
                pass

        def chain(gs):
            for g_ in gs:
                yield from g_

        run_all(tile_A(0))
        run_all(tile_B(0))
        for i in range(NT):
            tile_C(i)
            parts = []
            if i >= 1:
                parts += [tile_D(i - 1), tile_E(i - 1)]
            if i + 1 < NT:
                parts += [tile_A(i + 1), tile_B(i + 1)]
            g = chain(parts)
            if 128 * (i + 1) > TOPK:
                bisect(i, g)
            run_all(g)
            tile_C2(i)
        run_all(tile_D(NT - 1))
        run_all(tile_E(NT - 1))
        S.barrier()

def build(ntok=SEQ, phases=("ffn1", "mix", "ffn2")):
    nc = bass.Bass("TRN2", target_bir_lowering=False)

    def din(name, shape):
        return nc.dram_tensor(name, shape, F32, kind="ExternalInput").ap()

    x = din("x", [ntok, D])
    w = {}
    for f in ("ffn1", "ffn2"):
        w[f] = dict(gpre=din("g_%s_pre" % f, [D]), wg=din("w_%s_gate" % f, [D, DFF]),
                    wu=din("w_%s_up" % f, [D, DFF]), wd=din("w_%s_down" % f, [DFF, D]),
                    gpost=din("g_%s_post" % f, [D]))
    wm = dict(g_mix_pre=din("g_mix_pre", [D]), w_in=din("w_in", [D, NIN]), w_gla_a2=din("w_gla_a2", [16, 256]),
              b_gla_a=din("b_gla_a", [256]), g_gla_norm=din("g_gla_norm", [128]), w_out=din("w_out", [D, D]),
              g_mix_post=din("g_mix_post", [D]))
    out = nc.dram_tensor("out", [ntok, D], F32, kind="ExternalOutput").ap()
    x1 = nc.dram_tensor("x1_scr", [ntok, D], F32, kind="Internal").ap()
    x2 = nc.dram_tensor("x2_scr", [ntok, D], F32, kind="Internal").ap()
    nb = ntok // 128
    with ExitStack() as es:
        S = Sync(nc, es)
        C = Consts(nc, S, es)
        cur, cur_b = x, bufs(nb)
        stages = [p for p in phases]
        for i, p in enumerate(stages):
            last = (i == len(stages) - 1)
            dst = out if last else (x1 if i == 0 else x2)
            dst_b = bufs(nb)
            if p in ("ffn1", "ffn2"):
                ww = w[p]
                ffn_phase(nc, S, C, p, cur, cur_b, dst, dst_b, ww["wg"], ww["wu"], ww["wd"],
                          ww["gpre"], ww["gpost"], ntok)
            else:
                mix_phase(nc, S, C, cur, cur_b, dst, dst_b, wm, ntok)
            cur, cur_b = dst, dst_b
        S.barrier()
    return nc


_NC_CACHE = {}


def kernel(**inputs):
    names = ["g_ffn1_pre", "w_ffn1_gate", "w_ffn1_up", "w_ffn1_down", "g_ffn1_post",
             "g_ffn2_pre", "w_ffn2_gate", "w_ffn2_up", "w_ffn2_down", "g_ffn2_post",
             "g_mix_pre", "w_in", "w_gla_a2", "b_gla_a", "g_gla_norm", "w_out", "g_mix_post"]
    x = np.ascontiguousarray(np.asarray(inputs["x"], dtype=np.float32))
    shared = {n: np.ascontiguousarray(np.asarray(inputs[n], dtype=np.float32)[0]) for n in names}
    nc = build()
    in_maps = []
    for c in range(NCORES):
        m = dict(shared)
        m["x"] = x[c]
        in_maps.append(m)
    res = run_bass_kernel_spmd(nc, in_maps, core_ids=list(range(NCORES)))
    return np.stack([np.asarray(r["out"]) for r in res.results], axis=0).astype(np.float32)
```

```python
import numpy as np
from contextlib import ExitStack
import concourse.bass as bass
import concourse.mybir as mybir
from concourse.bass_utils import run_bass_kernel_spmd

F32 = mybir.dt.float32
BF16 = mybir.dt.bfloat16
AF = mybir.ActivationFunctionType
ALU = mybir.AluOpType
AX = mybir.AxisListType

D = 1024
DFF = 2816
NFF = DFF // 128
KC = D // 128
EPS = 1e-6
SEQ = 8192
NCORES = 8


class Buf:
    __slots__ = ("w", "r", "x")

    def __init__(self, x=False):
        self.w = None
        self.r = {}
        self.x = x


class Eng:
    def __init__(self, S, name, eng, strict):
        self.S = S
        self.name = name
        self.eng = eng
        self.strict = strict
        self.sem = S.new_sem(name)
        self.count = 0
        self.seen = {}
        self.slots = None
        self.slot_i = 0


class Sync:
    SEM_ROLL = 30000
    NSLOT = 8

    def __init__(self, nc, es):
        self.nc = nc
        self.es = es
        self.nsem = 0
        self.pe = Eng(self, "pe", nc.tensor, False)
        self.act = Eng(self, "act", nc.scalar, True)
        self.dve = Eng(self, "dve", nc.vector, True)
        self.pool = Eng(self, "pool", nc.gpsimd, True)
        self.sp = Eng(self, "sp", nc.sync, False)
        self.engs = [self.pe, self.act, self.dve, self.pool, self.sp]
        for q in (self.sp, self.pool, self.act):
            q.slots = [[self.new_sem(q.name + "d%d" % i), 0] for i in range(self.NSLOT)]

    def new_sem(self, name):
        self.nsem += 1
        return self.es.enter_context(self.nc.semaphore("s%d_%s" % (self.nsem, name)))

    def _wait(self, E, tok):
        sem, val, owner = tok
        if owner is E and not E.strict:
            return
        k = id(sem)
        if E.seen.get(k, 0) >= val:
            return
        E.eng.wait_ge(sem, val)
        E.seen[k] = val

    def _deps(self, E, reads, writes):
        for b in reads:
            if b.w is not None:
                self._wait(E, b.w)
            if b.x:
                for tok in b.r.values():
                    if tok[2] is not E:
                        self._wait(E, tok)
        for b in writes:
            if b.w is not None:
                self._wait(E, b.w)
            for tok in b.r.values():
                self._wait(E, tok)

    def _mark(self, tok, reads, writes):
        k = id(tok[0])
        for b in reads:
            b.r[k] = tok
        for b in writes:
            b.w = tok
            b.r = {}

    def op(self, E, fn, reads=(), writes=()):
        self._deps(E, reads, writes)
        if E.count >= self.SEM_ROLL:
            E.sem = self.new_sem(E.name)
            E.count = 0
        ins = fn(E.eng)
        E.count += 1
        ins.then_inc(E.sem, 1)
        self._mark((E.sem, E.count, E), reads, writes)

    def dma(self, Q, out, in_, reads=(), writes=(), **kw):
        self._deps(Q, reads, writes)
        slot = Q.slots[Q.slot_i % self.NSLOT]
        Q.slot_i += 1
        if slot[1] > 0:
            self._wait(Q, (slot[0], slot[1], None))
        if slot[1] >= self.SEM_ROLL:
            slot[0] = self.new_sem(Q.name + "d")
            slot[1] = 0
        slot[1] += 16
        Q.eng.dma_start(out=out, in_=in_, **kw).then_inc(slot[0], 16)
        self._mark((slot[0], slot[1], None), reads, writes)

    def barrier(self):
        toks = [(E.sem, E.count, None) for E in self.engs if E.count > 0]
        for q in (self.sp, self.pool, self.act):
            for s in q.slots:
                if s[1] > 0:
                    toks.append((s[0], s[1], None))
        for E in self.engs:
            for t in toks:
                if t[0] is E.sem:
                    continue
                self._wait(E, t)


def bufs(n, x=False):
    return [Buf(x) for _ in range(n)]


class Consts:
    def __init__(self, nc, S, es):
        self.b = Buf()
        self.idf = es.enter_context(nc.sbuf_tensor("c_idf", [128, 128], F32))
        self.idb = es.enter_context(nc.sbuf_tensor("c_idb", [128, 128], BF16))
        self.mhalf = es.enter_context(nc.sbuf_tensor("c_mhalf", [128, 1], F32))
        S.op(S.pool, lambda e: e.memset(self.idf[:], 1.0), writes=[self.b])
        S.op(S.pool, lambda e: e.affine_select(out=self.idf[:], in_=self.idf[:], pattern=[[-1, 128]],
                                              compare_op=ALU.is_equal, fill=0.0, base=0,
                                              channel_multiplier=1), writes=[self.b])
        S.op(S.pool, lambda e: e.memset(self.mhalf[:], -0.5), writes=[self.b])
        S.op(S.dve, lambda e: e.tensor_copy(out=self.idb[:], in_=self.idf[:]), writes=[self.b])


def rstd_ops(S, C, ss, tmp, out, n, post_scale, rb, wb, dim=D):
    S.op(S.pool, lambda e: e.tensor_scalar(out=tmp, in0=ss, scalar1=1.0 / dim, scalar2=EPS,
                                          op0=ALU.mult, op1=ALU.add), reads=[rb], writes=[wb])
    S.op(S.pool, lambda e: e.tensor_tensor(out=out, in0=tmp, in1=C.mhalf[:].to_broadcast([128, n]),
                                          op=ALU.pow), reads=[C.b], writes=[wb])
    if post_scale != 1.0:
        S.op(S.pool, lambda e: e.tensor_scalar(out=out, in0=out, scalar1=post_scale, scalar2=0.0,
                                              op0=ALU.mult, op1=ALU.add), writes=[wb])


def ffn_phase(nc, S, C, tag, xi, xi_b, xo, xo_b, wg_d, wu_d, wd_d, gpre_d, gpost_d, ntok):
    T = 256
    NT = ntok // T
    with ExitStack() as es:
        def sb(name, shape, dt):
            return es.enter_context(nc.sbuf_tensor(tag + name, shape, dt))

        def ps(name, shape, dt):
            return es.enter_context(nc.psum_tensor(tag + name, shape, dt))

        wg = sb("wg", [128, KC, DFF], BF16)
        wu = sb("wu", [128, KC, DFF], BF16)
        wd = sb("wd", [128, NFF, D], BF16)
        gcol = sb("gcol", [128, KC], F32)
        gpost = sb("gpost", [128, D], F32)
        xin = [sb("xin%d" % i, [128, 2, D], F32) for i in range(2)]
        hT = [sb("hT%d" % i, [128, KC, T], BF16) for i in range(2)]
        xs = [sb("xs%d" % i, [128, D], BF16) for i in range(2)]
        junk = sb("junk", [128, D], BF16)
        sg = [sb("sg%d" % i, [128, T], F32) for i in range(2)]
        actT = [sb("actT%d" % i, [128, T], BF16) for i in range(3)]
        tmp = [sb("tmp%d" % i, [128, D], F32) for i in range(2)]
        st = sb("st", [128, 16], F32)
        ptr = [ps("ptr%d" % i, [128, KC, 128], BF16) for i in range(2)]
        pgu = [ps("pgu%d" % i, [128, 512], F32) for i in range(2)]
        pd = [ps("pd%d" % i, [128, D], F32) for i in range(2)]

        b_wg, b_wu, b_wd, b_g = Buf(), Buf(), Buf(), Buf()
        b_xin = [bufs(2) for _ in range(2)]
        b_hT = [bufs(2) for _ in range(2)]
        b_xs, b_junk, b_sg, b_actT, b_tmp = bufs(2), Buf(), bufs(2), bufs(3), bufs(2)
        b_st = [[Buf() for _ in range(2)] for _ in range(2)]
        b_st2 = bufs(2)
        b_ptr, b_pgu, b_pd = bufs(2, True), bufs(2, True), bufs(2, True)

        wg_v = wg_d.rearrange("(kc p) n -> p kc n", p=128)
        wu_v = wu_d.rearrange("(kc p) n -> p kc n", p=128)
        wd_v = wd_d.rearrange("(fc p) n -> p fc n", p=128)
        for kc in range(KC):
            for h in range(2):
                sl = slice(h * (DFF // 2), (h + 1) * (DFF // 2))
                S.dma(S.pool, wg[:, kc, sl], wg_v[:, kc, sl], writes=[b_wg])
                S.dma(S.pool, wu[:, kc, sl], wu_v[:, kc, sl], writes=[b_wu])
        for fc in range(NFF):
            S.dma(S.pool, wd[:, fc, :], wd_v[:, fc, :], writes=[b_wd])
        with nc.allow_non_contiguous_dma(reason="tiny gain vector transpose load"):
            S.dma(S.sp, gcol[:], gpre_d.rearrange("(kc p) -> p kc", p=128), writes=[b_g])
        S.dma(S.sp, gpost[:], gpost_d.unsqueeze(0).to_broadcast([128, D]), writes=[b_g])

        def load(t):
            par = t % 2
            for s in range(2):
                r0 = t * T + s * 128
                S.dma(S.sp, xin[par][:, s, :], xi[r0:r0 + 128, :],
                      reads=[xi_b[r0 // 128]], writes=[b_xin[par][s]])

        def prologue(t):
            par = t % 2
            for s in range(2):
                q = s
                stt = st[:, (par * 2 + s) * 3:(par * 2 + s) * 3 + 3]
                S.op(S.act, lambda e: e.activation(out=junk[:], in_=xin[par][:, s, :], func=AF.Square,
                                                   accum_out=stt[:, 0:1]),
                     reads=[b_xin[par][s]], writes=[b_junk, b_st[par][s]])
                rstd_ops(S, C, stt[:, 0:1], stt[:, 1:2], stt[:, 2:3], 1, 1.0, b_st[par][s], b_st[par][s])
                S.op(S.dve, lambda e: e.tensor_scalar(out=xs[q][:], in0=xin[par][:, s, :],
                                                      scalar1=stt[:, 2:3], scalar2=None, op0=ALU.mult),
                     reads=[b_xin[par][s], b_st[par][s]], writes=[b_xs[q]])
                for kc in range(KC):
                    S.op(S.pe, lambda e: e.transpose(ptr[q][:, kc, :], xs[q][:, kc * 128:(kc + 1) * 128],
                                                     C.idb[:]),
                         reads=[b_xs[q], C.b], writes=[b_ptr[q]])
                S.op(S.dve, lambda e: e.tensor_tensor(out=hT[par][:, :, s * 128:(s + 1) * 128], in0=ptr[q][:],
                                                      in1=gcol[:].unsqueeze(2).to_broadcast([128, KC, 128]),
                                                      op=ALU.mult),
                     reads=[b_ptr[q], b_g], writes=[b_hT[par][s]])

        def GU(t, fc):
            par = t % 2
            pb = fc % 2
            for (w, bw, off) in ((wg, b_wg, 0), (wu, b_wu, T)):
                for kc in range(KC):
                    S.op(S.pe, lambda e: e.matmul(pgu[pb][:, off:off + T], lhsT=w[:, kc, fc * 128:(fc + 1) * 128],
                                                  rhs=hT[par][:, kc, :], start=(kc == 0), stop=(kc == KC - 1)),
                         reads=[bw, b_hT[par][0], b_hT[par][1]], writes=[b_pgu[pb]])

        def ACTF(t, fc):
            pb = fc % 2
            r = fc % 3
            S.op(S.act, lambda e: e.activation(out=sg[pb][:], in_=pgu[pb][:, 0:T], func=AF.Silu),
                 reads=[b_pgu[pb]], writes=[b_sg[pb]])
            S.op(S.dve, lambda e: e.tensor_tensor(out=actT[r][:], in0=pgu[pb][:, T:2 * T], in1=sg[pb][:],
                                                  op=ALU.mult),
                 reads=[b_pgu[pb], b_sg[pb]], writes=[b_actT[r]])

        def DN(t, fc):
            r = fc % 3
            for s in range(2):
                for h in range(2):
                    S.op(S.pe, lambda e: e.matmul(pd[s][:, h * 512:(h + 1) * 512],
                                                  lhsT=actT[r][:, s * 128:(s + 1) * 128],
                                                  rhs=wd[:, fc, h * 512:(h + 1) * 512],
                                                  start=(fc == 0), stop=(fc == NFF - 1)),
                         reads=[b_actT[r], b_wd], writes=[b_pd[s]])

        def epilogue(t):
            par = t % 2
            for s in range(2):
                stt = st[:, 12 + s * 2:12 + s * 2 + 2]
                S.op(S.act, lambda e: e.activation(out=junk[:], in_=pd[s][:], func=AF.Square,
                                                   accum_out=stt[:, 0:1]),
                     reads=[b_pd[s]], writes=[b_junk, b_st2[s]])
                rstd_ops(S, C, stt[:, 0:1], stt[:, 1:2], stt[:, 1:2], 1, 0.5, b_st2[s], b_st2[s])
                S.op(S.dve, lambda e: e.scalar_tensor_tensor(out=tmp[s][:], in0=pd[s][:], scalar=stt[:, 1:2],
                                                             in1=gpost[:], op0=ALU.mult, op1=ALU.mult),
                     reads=[b_pd[s], b_st2[s], b_g], writes=[b_tmp[s]])
                S.op(S.pool, lambda e: e.tensor_tensor(out=xin[par][:, s, :], in0=xin[par][:, s, :],
                                                       in1=tmp[s][:], op=ALU.add),
                     reads=[b_tmp[s]], writes=[b_xin[par][s]])
                r0 = t * T + s * 128
                S.dma(S.sp, xo[r0:r0 + 128, :], xin[par][:, s, :],
                      reads=[b_xin[par][s]], writes=[xo_b[r0 // 128]])

        load(0)
        prologue(0)
        for t in range(NT):
            if t + 1 < NT:
                load(t + 1)
            GU(t, 0)
            for fc in range(NFF):
                if fc + 1 < NFF:
                    GU(t, fc + 1)
                if fc == 8 and t + 1 < NT:
                    prologue(t + 1)
                ACTF(t, fc)
                DN(t, fc)
            epilogue(t)
        S.barrier()


O_AQ, O_AK, O_AV, O_IQ, O_IK, O_IW, O_GQ, O_GK, O_GV, O_GA, O_GG = (
    0, 512, 640, 768, 1280, 1344, 1352, 1608, 1864, 2376, 2392)
NIN = 2904
NIT = 16
SCHED = {10: 1, 11: 2, 12: 3, 13: 4, 14: 5, 15: 6}
BIG = 240000.0
IWS = (8.0 ** -0.5) * (64.0 ** -0.5)


def mix_phase(nc, S, C, xi, xi_b, xo, xo_b, W, ntok):
    NT = ntok // 128
    TOPK = min(256, ntok // 4)
    SC_W = ntok
    CMB_W = max(ntok, 8192)
    with ExitStack() as es:
        def sb(name, shape, dt):
            return es.enter_context(nc.sbuf_tensor("mx" + name, shape, dt))

        def ps(name, shape, dt):
            return es.enter_context(nc.psum_tensor("mx" + name, shape, dt))

        win = sb("win", [128, KC, NIN], BF16)
        cmb = sb("cmb", [128, CMB_W], BF16)
        woa = cmb[0:64, 0:8192].rearrange("p (h n) -> p h n", h=8)
        wog = sb("wog", [128, 4, D], BF16)
        wa2 = sb("wa2", [16, 256], BF16)
        KT = sb("KT", [128, ntok], BF16)
        Vaug = sb("Vaug", [128, NT, 2, 65], BF16)
        sc = sb("sc", [128, SC_W], F32)
        MB = sb("MB", [128, ntok], BF16)
        gcol = sb("gcol", [128, KC], F32)
        gpost = sb("gpost", [128, D], F32)
        gnorm = sb("gnorm", [128, 128], F32)
        babc = sb("babc", [128, 256], F32)
        M1s = sb("M1s", [128, 16], F32)
        Gsel = sb("Gsel", [128, 8], F32)
        I4 = sb("I4", [128, 4, 128], BF16)
        TriU = sb("TriU", [128, 128], F32)
        TriL2 = sb("TriL2", [128, 128], F32)
        CM = sb("CM", [128, 128], F32)
        pow2 = sb("pow2", [128, NIT + 1], F32)
        ones32 = sb("ones32", [128, 64], F32)
        thr_all = sb("thrall", [128, 1], F32)
        xt = sb("xt", [128, D], F32)
        xs = sb("xs", [128, D], BF16)
        hT = sb("hT", [128, KC, 128], BF16)
        QTs = [sb("QT%d" % k, [128, 8, 128], BF16) for k in range(2)]
        IQT = sb("IQT", [128, 8, 8, 16], BF16)
        IQTv = IQT[64:128, :, :, :]
        OTt = sb("OTt", [64, 1024], BF16)
        gqT = sb("gqT", [128, 256], F32)
        gkT = sb("gkT", [128, 256], F32)
        gaT = sb("gaT", [16, 128], BF16)
        iwtok = sb("iwtok", [128, 8], F32)
        gktok = sb("gktok", [128, 256], F32)
        gvbf = sb("gvbf", [128, 512], BF16)
        ggs = xt[:, 512:1024]
        zs = sb("zs", [128, 256], F32)
        E1 = sb("E1", [128, 2, 128], F32)
        E2 = sb("E2", [128, 256], F32)
        E3 = sb("E3", [128, 256], F32)
        qd = sb("qd", [128, 2, 128], BF16)
        kin = sb("kin", [128, 2, 128], BF16)
        kout = sb("kout", [128, 256], BF16)
        AT = [sb("AT%d" % k, [128, 128], BF16) for k in range(2)]
        Sst = sb("Sst", [128, 2, 128], F32)
        S0 = sb("S0", [128, 2, 128], BF16)
        S1 = sb("S1", [128, 2, 128], BF16)
        junkg = sb("junkg", [128, 128], BF16)
        ogla = sb("ogla", [128, 4, 128], BF16)
        ogTs = [sb("ogT%d" % k, [128, 4, 128], BF16) for k in range(2)]
        nrm = xt[:, 0:512]
        eg = nrm
        cj = sb("cj", [128, ntok], mybir.dt.uint8)
        Wsel = sb("Wsel", [128, 8, 128], BF16)
        L = sb("L", [128, 8, 16], F32)
        iwcol = sb("iwcol", [128, 8], F32)
        R = [sb("R%d" % k, [128, 512], BF16) for k in range(3)]
        NPT = 2
        PT = [sb("PT%d" % k, [128, 512], BF16) for k in range(NPT)]
        OT = [OTt[:, k * 512:(k + 1) * 512] for k in range(2)]
        st = sb("st", [128, 16], F32)
        bs = sb("bs", [128, 4], F32)
        hst = sb("hst", [128, NIT + 1], F32)
        mid = sb("mid", [128, 1], F32)
        cnt = sb("cnt", [128, 1], F32)
        gp = sb("gp", [128, 1], F32)
        thr = sb("thr", [128, 1], F32)

        ptr = ps("ptr", [128, KC, 128], BF16)
        pool = [ps("pb%d" % k, [128, 512], F32) for k in range(5)]
        acc = ps("acc", [128, 1024], F32)

        b_pool = bufs(5, True)
        b_acc = bufs(2, True)
        b_ptr, b_win, b_woa, b_wog, b_c = Buf(True), Buf(), Buf(), Buf(), Buf()
        b_KT, b_IKT, b_V = bufs(NT), bufs(NT), bufs(NT)
        b_sc, b_MB = Buf(), Buf()
        b_xts, b_QTs, b_ogTs, b_nrm, b_cj, b_Wsel = bufs(2), bufs(2), bufs(2), Buf(), Buf(), Buf()
        (b_xt, b_xs, b_hT, b_QT, b_IQT, b_gqT, b_gkT, b_gaT, b_iw, b_gktok, b_gv, b_nrm, b_gg, b_zs, b_E1,
         b_E2, b_E3, b_qd, b_kin, b_kout, b_Sst, b_S0, b_S1, b_junkg, b_ogla, b_ogT, b_L, b_iwcol, b_st,
         b_stg, b_bs, b_hst, b_mid, b_cnt, b_gp, b_thr) = bufs(36)
        b_AT, b_R, b_PT, b_OT = bufs(2), bufs(3), bufs(NPT), bufs(2)
        b_nrm = b_xt
        b_gg = b_xt

        rc = {"a": 0, "i": 0, "p": 0}

        def rot(key, lo, n):
            k = lo + rc[key] % n
            rc[key] += 1
            return pool[k], b_pool[k]

        win_v = W["w_in"].rearrange("(kc p) n -> p kc n", p=128)
        hw = NIN // 2
        for kc in range(KC):
            for h in range(2):
                S.dma(S.pool, win[:, kc, h * hw:(h + 1) * hw], win_v[:, kc, h * hw:(h + 1) * hw], writes=[b_win])
        for h in range(8):
            S.dma(S.pool, cmb[0:64, h * 1024:(h + 1) * 1024], W["w_out"][h * 64:(h + 1) * 64, :], writes=[b_woa])
        for hg in range(4):
            S.dma(S.pool, wog[:, hg, :], W["w_out"][512 + hg * 128:512 + (hg + 1) * 128, :], writes=[b_wog])
        S.dma(S.pool, wa2[:], W["w_gla_a2"], writes=[b_c])
        with nc.allow_non_contiguous_dma(reason="tiny gain vector transpose load"):
            S.dma(S.sp, gcol[:], W["g_mix_pre"].rearrange("(kc p) -> p kc", p=128), writes=[b_c])
        S.dma(S.sp, gpost[:], W["g_mix_post"].unsqueeze(0).to_broadcast([128, D]), writes=[b_c])
        S.dma(S.sp, gnorm[:], W["g_gla_norm"].unsqueeze(0).to_broadcast([128, 128]), writes=[b_c])
        S.dma(S.sp, babc[:], W["b_gla_a"].unsqueeze(0).to_broadcast([128, 256]), writes=[b_c])

        P = S.pool
        S.op(P, lambda e: e.memset(Vaug[:], 1.0), writes=b_V)
        S.op(P, lambda e: e.memset(Sst[:], 0.0), writes=[b_Sst])
        S.op(P, lambda e: e.memset(S0[:], 0.0), writes=[b_S0])
        S.op(P, lambda e: e.memset(thr_all[:], -1e29), writes=[b_c])
        for k in range(2):
            S.op(P, lambda e: e.memset(QTs[k][:], 0.0), writes=[b_QTs[k]])
        S.op(P, lambda e: e.memset(IQT[:], 0.0), writes=[b_IQT])
        S.op(P, lambda e: e.memset(ones32[:], 1.0), writes=[b_c])
        S.op(P, lambda e: e.memset(Wsel[:], 0.0), writes=[b_Wsel])
        for k in range(NIT + 1):
            S.op(P, lambda e: e.memset(pow2[:, k:k + 1], 2.0 ** -(k + 1)), writes=[b_c])
        S.op(P, lambda e: e.memset(TriU[:], 1.0), writes=[b_c])
        S.op(P, lambda e: e.affine_select(out=TriU[:], in_=TriU[:], pattern=[[1, 128]], compare_op=ALU.is_ge,
                                          fill=0.0, base=0, channel_multiplier=-1), writes=[b_c])
        S.op(P, lambda e: e.memset(TriU[0:64, 64:128], 0.0), writes=[b_c])
        S.op(P, lambda e: e.memset(TriL2[:], 1.0), writes=[b_c])
        S.op(P, lambda e: e.affine_select(out=TriL2[:], in_=TriL2[:], pattern=[[-1, 128]], compare_op=ALU.is_ge,
                                          fill=0.0, base=-1, channel_multiplier=1), writes=[b_c])
        S.op(P, lambda e: e.memset(TriL2[64:128, 0:64], 0.0), writes=[b_c])
        S.op(P, lambda e: e.memset(CM[:], 0.0), writes=[b_c])
        S.op(P, lambda e: e.affine_select(out=CM[:], in_=CM[:], pattern=[[-1, 128]], compare_op=ALU.is_ge,
                                          fill=-1e30, base=0, channel_multiplier=1), writes=[b_c])
        V_ = S.dve
        S.op(V_, lambda e: e.tensor_reduce(out=M1s[:], in_=C.idf[:].rearrange("p (j t) -> p t j", t=16),
                                           axis=AX.X, op=ALU.add), reads=[C.b], writes=[b_c])
        S.op(V_, lambda e: e.tensor_reduce(out=Gsel[:], in_=C.idf[:].rearrange("p (j t) -> p j t", t=16),
                                           axis=AX.X, op=ALU.add), reads=[C.b], writes=[b_c])
        S.op(V_, lambda e: e.tensor_copy(out=I4[:], in_=C.idb[:].unsqueeze(1).to_broadcast([128, 4, 128])),
             reads=[C.b], writes=[b_c])

        def fm(bank, bb, slot, c0, M, pbase=0):
            for kc in range(KC):
                S.op(S.pe, lambda e: e.matmul(bank[pbase:pbase + M, slot * 128:(slot + 1) * 128],
                                              lhsT=win[:, kc, c0:c0 + M], rhs=hT[:, kc, :],
                                              start=(kc == 0), stop=(kc == KC - 1)),
                     reads=[b_win, b_hT], writes=[bb])

        def tm(bank, bb, o0, c0, N):
            for kc in range(KC):
                S.op(S.pe, lambda e: e.matmul(bank[:, o0:o0 + N], lhsT=hT[:, kc, :], rhs=win[:, kc, c0:c0 + N],
                                              start=(kc == 0), stop=(kc == KC - 1)),
                     reads=[b_win, b_hT], writes=[bb])

        def tile_A(i):
            r0 = i * 128
            QT, b_QT = QTs[i % 2], b_QTs[i % 2]
            S.dma(S.sp, xt[:], xi[r0:r0 + 128, :], reads=[xi_b[i]], writes=[b_xt])
            S.op(S.act, lambda e: e.activation(out=xs[:], in_=xt[:], func=AF.Square, accum_out=st[:, 0:1]),
                 reads=[b_xt], writes=[b_xs, b_st])
            rstd_ops(S, C, st[:, 0:1], st[:, 1:2], st[:, 2:3], 1, 1.0, b_st, b_st)
            S.op(S.dve, lambda e: e.tensor_scalar(out=xs[:], in0=xt[:], scalar1=st[:, 2:3], scalar2=None,
                                                  op0=ALU.mult), reads=[b_xt, b_st], writes=[b_xs])
            for kc in range(KC):
                S.op(S.pe, lambda e: e.transpose(ptr[:, kc, :], xs[:, kc * 128:(kc + 1) * 128], C.idb[:]),
                     reads=[b_xs, C.b], writes=[b_ptr])
            S.op(S.dve, lambda e: e.tensor_tensor(out=hT[:], in0=ptr[:],
                                                  in1=gcol[:].unsqueeze(2).to_broadcast([128, KC, 128]),
                                                  op=ALU.mult), reads=[b_ptr, b_c], writes=[b_hT])
            yield
            bk, bb = rot("a", 0, 5)
            for r in range(4):
                fm(bk, bb, r, O_AQ + r * 64, 64, 0)
                fm(bk, bb, r, O_AQ + (r + 4) * 64, 64, 64)
            S.op(S.act, lambda e: e.copy(out=QT[0:64, 0:4, :].rearrange("p r t -> p (r t)"), in_=bk[0:64, :]),
                 reads=[bb], writes=[b_QT])
            S.op(S.act, lambda e: e.copy(out=QT[64:128, 4:8, :].rearrange("p r t -> p (r t)"), in_=bk[64:128, :]),
                 reads=[bb], writes=[b_QT])
            for hb in range(2):
                yield
                bk, bb = rot("a", 0, 5)
                for hh in range(4):
                    fm(bk, bb, hh, O_IQ + (hb * 4 + hh) * 64, 64, 64)
                for hh in range(4):
                    S.op(S.act, lambda e: e.copy(out=IQTv[:, :, hb * 4 + hh, :],
                                                 in_=bk[64:128, hh * 128:(hh + 1) * 128].rearrange(
                                                     "p (j t) -> p j t", t=16)),
                         reads=[bb], writes=[b_IQT])
            yield
            bk, bb = rot("a", 0, 5)
            fm(bk, bb, 0, O_AK, 128)
            fm(bk, bb, 1, O_IK, 64, 64)
            fm(bk, bb, 2, O_GQ, 128)
            fm(bk, bb, 3, O_GQ + 128, 128)
            S.op(S.dve, lambda e: e.tensor_copy(out=KT[:, r0:r0 + 128], in_=bk[:, 0:128]),
                 reads=[bb], writes=[b_KT[i]])
            S.op(S.dve, lambda e: e.tensor_copy(out=cmb[64:128, r0:r0 + 128], in_=bk[64:128, 128:256]),
                 reads=[bb], writes=[b_IKT[i]])
            S.op(S.act, lambda e: e.copy(out=gqT[:], in_=bk[:, 256:512]), reads=[bb], writes=[b_gqT])
            yield
            bk, bb = rot("a", 0, 5)
            fm(bk, bb, 0, O_GK, 128)
            fm(bk, bb, 1, O_GK + 128, 128)
            fm(bk, bb, 2, O_GA, 16)
            S.op(S.act, lambda e: e.copy(out=gkT[:], in_=bk[:, 0:256]), reads=[bb], writes=[b_gkT])
            S.op(S.dve, lambda e: e.tensor_copy(out=gaT[:], in_=bk[0:16, 256:384]), reads=[bb], writes=[b_gaT])
            yield
            bk, bb = rot("a", 0, 5)
            tm(bk, bb, 0, O_AV, 128)
            tm(bk, bb, 128, O_IW, 8)
            tm(bk, bb, 136, O_GK, 256)
            S.op(S.dve, lambda e: e.tensor_copy(out=Vaug[:, i, :, 0:64],
                                                in_=bk[:, 0:128].rearrange("p (g d) -> p g d", g=2)),
                 reads=[bb], writes=[b_V[i]])
            S.op(S.dve, lambda e: e.tensor_scalar(out=iwtok[:], in0=bk[:, 128:136], scalar1=IWS, scalar2=None,
                                                  op0=ALU.mult), reads=[bb], writes=[b_iw])
            S.op(S.act, lambda e: e.copy(out=gktok[:], in_=bk[:, 136:392]), reads=[bb], writes=[b_gktok])
            yield
            bk, bb = rot("a", 0, 5)
            tm(bk, bb, 0, O_GV, 512)
            S.op(S.dve, lambda e: e.tensor_copy(out=gvbf[:], in_=bk[:, :]), reads=[bb], writes=[b_gv])
            yield
            bk, bb = rot("a", 0, 5)
            tm(bk, bb, 0, O_GG, 512)
            S.op(S.act, lambda e: e.activation(out=eg[:], in_=bk[:, :], func=AF.Exp, scale=-1.0),
                 reads=[bb], writes=[b_nrm])
            S.op(S.dve, lambda e: e.tensor_copy(out=ggs[:], in_=bk[:, :]), reads=[bb], writes=[b_gg])
            S.op(S.act, lambda e: e.activation(out=eg[:], in_=eg[:], func=AF.Ln, bias=1.0), writes=[b_nrm])
            S.op(S.act, lambda e: e.activation(out=eg[:], in_=eg[:], func=AF.Exp, scale=-1.0), writes=[b_nrm])
            S.op(S.pool, lambda e: e.tensor_tensor(out=ggs[:], in0=ggs[:], in1=eg[:], op=ALU.mult),
                 reads=[b_nrm], writes=[b_gg])
            S.op(S.pool, lambda e: e.tensor_tensor(out=ggs[:].rearrange("p (h v) -> p h v", h=4),
                                                   in0=ggs[:].rearrange("p (h v) -> p h v", h=4),
                                                   in1=gnorm[:].unsqueeze(1).to_broadcast([128, 4, 128]),
                                                   op=ALU.mult), reads=[b_c], writes=[b_gg])

        def oreg(h):
            f, a = h // 2, h % 2
            return acc[:, a * 512 + f * 128:a * 512 + (f + 1) * 128]

        def tile_B(i):
            ogT, b_ogT = ogTs[i % 2], b_ogTs[i % 2]
            bk, bb = rot("a", 0, 5)
            S.op(S.pe, lambda e: e.matmul(bk[:, 0:256], lhsT=gaT[:], rhs=wa2[:], start=True, stop=True),
                 reads=[b_gaT, b_c], writes=[bb])
            S.op(S.dve, lambda e: e.tensor_tensor(out=zs[:], in0=bk[:, 0:256], in1=babc[:], op=ALU.add),
                 reads=[bb, b_c], writes=[b_zs])
            S.op(S.act, lambda e: e.activation(out=zs[:], in_=zs[:], func=AF.Exp, scale=-1.0), writes=[b_zs])
            S.op(S.act, lambda e: e.activation(out=zs[:], in_=zs[:], func=AF.Ln, bias=1.0), writes=[b_zs])
            yield
            cb, cbb = rot("a", 0, 5)
            for f in range(2):
                S.op(S.pe, lambda e: e.matmul(cb[:, f * 128:(f + 1) * 128], lhsT=zs[:, f * 128:(f + 1) * 128],
                                              rhs=TriU[:], start=True, stop=True),
                     reads=[b_zs, b_c], writes=[cbb])
            S.op(S.pe, lambda e: e.matmul(cb[:, 256:512], lhsT=TriL2[:], rhs=zs[:], start=True, stop=True),
                 reads=[b_zs, b_c], writes=[cbb])
            S.op(S.act, lambda e: e.activation(out=E1[:].rearrange("p f t -> p (f t)"), in_=cb[:, 0:256],
                                               func=AF.Exp, scale=-1.0 / 16), reads=[cbb], writes=[b_E1])
            S.op(S.act, lambda e: e.activation(out=E2[:], in_=cb[:, 0:256], func=AF.Exp, scale=1.0 / 16),
                 reads=[cbb], writes=[b_E2])
            S.op(S.act, lambda e: e.activation(out=E3[:], in_=cb[:, 256:512], func=AF.Exp, scale=-1.0 / 16),
                 reads=[cbb], writes=[b_E3])
            yield
            S.op(S.dve, lambda e: e.scalar_tensor_tensor(out=qd[:].rearrange("p f t -> p (f t)"), in0=gqT[:],
                                                         scalar=0.125, in1=E1[:].rearrange("p f t -> p (f t)"),
                                                         op0=ALU.mult, op1=ALU.mult),
                 reads=[b_gqT, b_E1], writes=[b_qd])
            S.op(S.dve, lambda e: e.tensor_tensor(out=kin[:].rearrange("p f t -> p (f t)"), in0=gkT[:], in1=E2[:],
                                                  op=ALU.mult), reads=[b_gkT, b_E2], writes=[b_kin])
            S.op(S.dve, lambda e: e.tensor_tensor(out=kout[:], in0=gktok[:], in1=E3[:], op=ALU.mult),
                 reads=[b_gktok, b_E3], writes=[b_kout])

            def state_step(c, Sdst, b_Sdst):
                ub, ubb = rot("a", 0, 5)
                for h in range(4):
                    f, a = h // 2, h % 2
                    S.op(S.pe, lambda e: e.matmul(ub[a * 64:(a + 1) * 64, f * 128:(f + 1) * 128],
                                                  lhsT=kout[c * 64:(c + 1) * 64, h * 64:(h + 1) * 64],
                                                  rhs=gvbf[c * 64:(c + 1) * 64, h * 128:(h + 1) * 128],
                                                  start=True, stop=True),
                         reads=[b_kout, b_gv], writes=[ubb])
                for f in range(2):
                    S.op(S.dve, lambda e: e.scalar_tensor_tensor(out=Sst[:, f, :], in0=Sst[:, f, :],
                                                                 scalar=E1[:, f, c * 64 + 63:c * 64 + 64],
                                                                 in1=ub[:, f * 128:(f + 1) * 128],
                                                                 op0=ALU.mult, op1=ALU.add),
                         reads=[ubb, b_E1], writes=[b_Sst])
                S.op(S.act, lambda e: e.copy(out=Sdst[:].rearrange("p f v -> p (f v)"),
                                             in_=Sst[:].rearrange("p f v -> p (f v)")),
                     reads=[b_Sst], writes=[b_Sdst])

            yield
            state_step(0, S1, b_S1)
            for h in range(4):
                yield
                f, a = h // 2, h % 2
                rows = slice(a * 64, (a + 1) * 64)
                ab, abb = rot("a", 0, 5)
                S.op(S.pe, lambda e: e.matmul(ab[:, 0:128], lhsT=kin[rows, f, :], rhs=qd[rows, f, :],
                                              start=True, stop=True), reads=[b_kin, b_qd], writes=[abb])
                S.op(S.dve, lambda e: e.tensor_tensor(out=AT[h % 2][:], in0=ab[:, 0:128], in1=TriU[:], op=ALU.mult),
                     reads=[abb, b_c], writes=[b_AT[h % 2]])
                o_ = oreg(h)
                S.op(S.pe, lambda e: e.matmul(o_, lhsT=AT[h % 2][:], rhs=gvbf[:, h * 128:(h + 1) * 128],
                                              start=True, stop=False),
                     reads=[b_AT[h % 2], b_gv], writes=[b_acc[a]])
                S.op(S.pe, lambda e: e.matmul(o_[0:64, :], lhsT=qd[rows, f, 0:64], rhs=S0[rows, f, :],
                                              start=False, stop=True),
                     reads=[b_qd, b_S0], writes=[b_acc[a]])
                S.op(S.pe, lambda e: e.matmul(o_[64:128, :], lhsT=qd[rows, f, 64:128], rhs=S1[rows, f, :],
                                              start=False, stop=True),
                     reads=[b_qd, b_S1], writes=[b_acc[a]])
            yield
            state_step(1, S0, b_S0)
            yield
            for h in range(4):
                S.op(S.act, lambda e: e.activation(out=junkg[:], in_=oreg(h), func=AF.Square,
                                                   accum_out=st[:, 4 + h:5 + h]),
                     reads=[b_acc[h % 2]], writes=[b_junkg, b_stg])
            rstd_ops(S, C, st[:, 4:8], st[:, 8:12], st[:, 12:16], 4, 1.0, b_stg, b_stg, dim=128)
            yield
            for h in range(4):
                S.op(S.dve, lambda e: e.scalar_tensor_tensor(out=ogla[:, h, :], in0=oreg(h),
                                                             scalar=st[:, 12 + h:13 + h],
                                                             in1=ggs[:, h * 128:(h + 1) * 128],
                                                             op0=ALU.mult, op1=ALU.mult),
                     reads=[b_acc[h % 2], b_stg, b_gg], writes=[b_ogla])
            yield
            for h in range(4):
                S.op(S.pe, lambda e: e.transpose(ptr[:, h, :], ogla[:, h, :], C.idb[:]),
                     reads=[b_ogla, C.b], writes=[b_ptr])
            S.op(S.act, lambda e: e.copy(out=ogT[:], in_=ptr[:, 0:4, :]), reads=[b_ptr], writes=[b_ogT])

        def tile_C(i):
            NK = 128 * (i + 1)
            S.op(S.dve, lambda e: e.tensor_tensor(out=L[:], in0=iwtok[:].unsqueeze(2).to_broadcast([128, 8, 16]),
                                                  in1=M1s[:].unsqueeze(1).to_broadcast([128, 8, 16]),
                                                  op=ALU.mult), reads=[b_iw, b_c], writes=[b_L])
            wb_, wbb = pool[4], b_pool[4]
            S.op(S.pe, lambda e: e.matmul(wb_[:, 0:8], lhsT=L[:].rearrange("p h t -> p (h t)"), rhs=Gsel[:],
                                          start=True, stop=True), reads=[b_L, b_c], writes=[wbb])
            S.op(S.act, lambda e: e.copy(out=iwcol[:], in_=wb_[:, 0:8]), reads=[wbb], writes=[b_iwcol])
            for j in range(8):
                S.op(S.dve, lambda e: e.tensor_scalar(out=Wsel[:, j, 16 * j:16 * j + 16], in0=M1s[:],
                                                      scalar1=iwcol[:, j:j + 1], scalar2=None, op0=ALU.mult),
                     reads=[b_iwcol, b_c], writes=[b_Wsel])
            nkb = (NK + 511) // 512
            for kb in range(nkb):
                wdt = min(512, NK - kb * 512)
                kts = [b_IKT[t] for t in range(kb * 4, kb * 4 + wdt // 128)]
                sb_, sbb = pool[3 + kb % 2], b_pool[3 + kb % 2]

                def LG(j):
                    lb, lbb = rot("i", 0, 3)
                    S.op(S.pe, lambda e: e.matmul(lb[:, 0:wdt],
                                                  lhsT=IQT[:, j, :, :].rearrange("p h t -> p (h t)"),
                                                  rhs=cmb[:, kb * 512:kb * 512 + wdt],
                                                  start=True, stop=True), reads=[b_IQT, b_woa] + kts, writes=[lbb])
                    return lb, lbb

                lgq = [LG(0), LG(1)]
                for j in range(8):
                    if j + 2 < 8:
                        lgq.append(LG(j + 2))
                    lb, lbb = lgq[j]
                    r = j % 3
                    if j % 2 == 0:
                        S.op(S.act, lambda e: e.activation(out=R[r][:, 0:wdt], in_=lb[:, 0:wdt], func=AF.Relu),
                             reads=[lbb], writes=[b_R[r]])
                    else:
                        S.op(S.dve, lambda e: e.tensor_scalar(out=R[r][:, 0:wdt], in0=lb[:, 0:wdt], scalar1=0.0,
                                                              scalar2=None, op0=ALU.max),
                             reads=[lbb], writes=[b_R[r]])
                    S.op(S.pe, lambda e: e.matmul(sb_[:, 0:wdt], lhsT=Wsel[:, j, :], rhs=R[r][:, 0:wdt],
                                                  start=(j == 0), stop=(j == 7)),
                         reads=[b_R[r], b_Wsel], writes=[sbb])
                S.op(S.dve, lambda e: e.tensor_copy(out=sc[:, kb * 512:kb * 512 + wdt], in_=sb_[:, 0:wdt]),
                     reads=[sbb], writes=[b_sc])
            bis = NK > TOPK
            if bis:
                S.op(S.dve, lambda e: e.tensor_reduce(out=bs[:, 0:1], in_=sc[:, 0:NK], axis=AX.X, op=ALU.max),
                     reads=[b_sc], writes=[b_bs])
                S.op(S.dve, lambda e: e.tensor_reduce(out=bs[:, 1:2], in_=sc[:, 0:NK], axis=AX.X, op=ALU.min),
                     reads=[b_sc], writes=[b_bs])
                S.op(S.dve, lambda e: e.tensor_tensor(out=bs[:, 2:3], in0=bs[:, 0:1], in1=bs[:, 1:2],
                                                      op=ALU.subtract), writes=[b_bs])
                S.op(S.dve, lambda e: e.tensor_scalar(out=hst[:], in0=pow2[:], scalar1=bs[:, 2:3], scalar2=None,
                                                      op0=ALU.mult), reads=[b_bs, b_c], writes=[b_hst])
                S.op(S.dve, lambda e: e.tensor_tensor(out=mid[:], in0=bs[:, 1:2], in1=hst[:, 0:1], op=ALU.add),
                     reads=[b_bs, b_hst], writes=[b_mid])
            S.op(S.dve, lambda e: e.tensor_tensor(out=sc[:, NK - 128:NK], in0=sc[:, NK - 128:NK], in1=CM[:],
                                                  op=ALU.add), reads=[b_c], writes=[b_sc])

        def bisect(i, g):
            NK = 128 * (i + 1)
            if i >= 1:
                next(g, None)
            for k in range(NIT):
                S.op(S.dve, lambda e: e.tensor_scalar(out=cj[:, 0:NK], in0=sc[:, 0:NK], scalar1=mid[:, 0:1],
                                                      scalar2=None, op0=ALU.is_ge, op1=ALU.add,
                                                      accum_out=cnt[:, 0:1]),
                     reads=[b_sc, b_mid], writes=[b_cj, b_cnt])
                S.op(S.dve, lambda e: e.tensor_scalar(out=gp[:], in0=cnt[:], scalar1=float(TOPK),
                                                      scalar2=hst[:, k:k + 1], op0=ALU.is_ge, op1=ALU.mult),
                     reads=[b_cnt, b_hst], writes=[b_gp])
                S.op(S.dve, lambda e: e.scalar_tensor_tensor(out=mid[:], in0=mid[:],
                                                             scalar=hst[:, k + 1:k + 2], in1=gp[:],
                                                             op0=ALU.subtract, op1=ALU.add),
                     reads=[b_gp, b_hst], writes=[b_mid])
                for _ in range(SCHED.get(k, 0)):
                    next(g, None)
            S.op(S.dve, lambda e: e.tensor_tensor(out=thr[:], in0=mid[:], in1=hst[:, NIT:NIT + 1],
                                                  op=ALU.subtract), reads=[b_mid, b_hst], writes=[b_thr])

        def tile_C2(i):
            NK = 128 * (i + 1)
            tap = thr if NK > TOPK else thr_all
            S.op(S.dve, lambda e: e.tensor_scalar(out=MB[:, 0:NK], in0=sc[:, 0:NK], scalar1=tap[:, 0:1],
                                                  scalar2=-BIG, op0=ALU.is_lt, op1=ALU.mult),
                 reads=[b_sc, b_thr, b_c], writes=[b_MB])

        def tile_D(i):
            QT, b_QT = QTs[i % 2], b_QTs[i % 2]
            QT2 = QT[:].rearrange("p h t -> p (h t)")
            I42 = I4[:].rearrange("p r t -> p (r t)")

            def QK(kb2):
                res = []
                for g in range(2):
                    pb_, pbb = rot("p", 0, 4)
                    S.op(S.pe, lambda e: e.matmul(pb_[:, :], lhsT=KT[:, kb2 * 128:(kb2 + 1) * 128],
                                                  rhs=QT2[:, g * 512:(g + 1) * 512], start=True, stop=False),
                         reads=[b_KT[kb2], b_QT], writes=[pbb])
                    S.op(S.pe, lambda e: e.matmul(pb_[:, :], lhsT=MB[:, kb2 * 128:(kb2 + 1) * 128], rhs=I42,
                                                  start=False, stop=True), reads=[b_MB, b_c], writes=[pbb])
                    res.append((pb_, pbb))
                return res

            cur = QK(0)
            for kb2 in range(i + 1):
                nxt = QK(kb2 + 1) if kb2 < i else None
                for g in range(2):
                    pb_, pbb = cur[g]
                    r = (2 * kb2 + g) % NPT
                    S.op(S.act, lambda e: e.activation(out=PT[r][:], in_=pb_[:, :], func=AF.Exp, scale=0.125),
                         reads=[pbb], writes=[b_PT[r]])
                    S.op(S.pe, lambda e: e.matmul(acc[0:65, g * 512:(g + 1) * 512], lhsT=Vaug[:, kb2, g, :],
                                                  rhs=PT[r][:], start=(kb2 == 0), stop=(kb2 == i)),
                         reads=[b_V[kb2], b_PT[r]], writes=[b_acc[g]])
                cur = nxt
            p4, b_p4 = pool[4], b_pool[4]
            for g in range(2):
                yield
                rrow = nrm[64:65, :]
                S.op(S.act, lambda e: e.activation(out=rrow, in_=acc[64:65, g * 512:(g + 1) * 512], func=AF.Ln),
                     reads=[b_acc[g]], writes=[b_nrm])
                S.op(S.act, lambda e: e.activation(out=rrow, in_=rrow, func=AF.Exp, scale=-1.0), writes=[b_nrm])
                S.op(S.pe, lambda e: e.matmul(p4[0:64, :], lhsT=ones32[64:65, 0:64], rhs=rrow,
                                              start=True, stop=True), reads=[b_nrm, b_c], writes=[b_p4])
                S.op(S.act, lambda e: e.copy(out=nrm[0:64, :], in_=p4[0:64, :]),
                     reads=[b_p4], writes=[b_nrm])
                S.op(S.dve, lambda e: e.tensor_tensor(out=OT[g], in0=acc[0:64, g * 512:(g + 1) * 512],
                                                      in1=nrm[0:64, :], op=ALU.mult),
                     reads=[b_acc[g], b_nrm], writes=[b_OT[g]])

        def tile_E(i):
            r0 = i * 128
            ogT, b_ogT = ogTs[i % 2], b_ogTs[i % 2]
            for half in range(2):
                cols = slice(half * 512, (half + 1) * 512)
                for h in range(8):
                    g, r = h // 4, h % 4
                    S.op(S.pe, lambda e: e.matmul(acc[:, cols], lhsT=OT[g][:, r * 128:(r + 1) * 128],
                                                  rhs=woa[:, h, cols], start=(h == 0), stop=False),
                         reads=[b_OT[g], b_woa], writes=[b_acc[half]])
                for hg in range(4):
                    S.op(S.pe, lambda e: e.matmul(acc[:, cols], lhsT=ogT[:, hg, :], rhs=wog[:, hg, cols],
                                                  start=False, stop=(hg == 3)),
                         reads=[b_ogT, b_wog], writes=[b_acc[half]])
            yield
            S.op(S.act, lambda e: e.activation(out=xs[:], in_=acc[:, :], func=AF.Square, accum_out=st[:, 0:1]),
                 reads=[b_acc[0], b_acc[1]], writes=[b_xs, b_st])
            rstd_ops(S, C, st[:, 0:1], st[:, 1:2], st[:, 2:3], 1, 1.0, b_st, b_st)
            S.op(S.dve, lambda e: e.scalar_tensor_tensor(out=acc[:, :], in0=acc[:, :], scalar=st[:, 2:3],
                                                         in1=gpost[:], op0=ALU.mult, op1=ALU.mult),
                 reads=[b_st, b_c], writes=[b_acc[0], b_acc[1]])
            for half in range(2):
                yield
                cols = slice(half * 512, (half + 1) * 512)
                S.dma(S.sp, nrm[:], xi[r0:r0 + 128, cols], reads=[xi_b[i]], writes=[b_nrm])
                S.op(S.dve, lambda e: e.tensor_tensor(out=nrm[:], in0=acc[:, cols], in1=nrm[:], op=ALU.add),
                     reads=[b_acc[half]], writes=[b_nrm])
                S.dma(S.sp, xo[r0:r0 + 128, cols], nrm[:], reads=[b_nrm], writes=[xo_b[i]])

        def run_all(g):
            for _ in g:
                pass

        def chain(gs):
            for g_ in gs:
                yield from g_

        run_all(tile_A(0))
        run_all(tile_B(0))
        for i in range(NT):
            tile_C(i)
            parts = []
            if i >= 1:
                parts += [tile_D(i - 1), tile_E(i - 1)]
            if i + 1 < NT:
                parts += [tile_A(i + 1), tile_B(i + 1)]
            g = chain(parts)
            if 128 * (i + 1) > TOPK:
                bisect(i, g)
            run_all(g)
            tile_C2(i)
        run_all(tile_D(NT - 1))
        run_all(tile_E(NT - 1))
        S.barrier()

def build(ntok=SEQ, phases=("ffn1", "mix", "ffn2")):
    nc = bass.Bass("TRN2", target_bir_lowering=False)

    def din(name, shape):
        return nc.dram_tensor(name, shape, F32, kind="ExternalInput").ap()

    x = din("x", [ntok, D])
    w = {}
    for f in ("ffn1", "ffn2"):
        w[f] = dict(gpre=din("g_%s_pre" % f, [D]), wg=din("w_%s_gate" % f, [D, DFF]),
                    wu=din("w_%s_up" % f, [D, DFF]), wd=din("w_%s_down" % f, [DFF, D]),
                    gpost=din("g_%s_post" % f, [D]))
    wm = dict(g_mix_pre=din("g_mix_pre", [D]), w_in=din("w_in", [D, NIN]), w_gla_a2=din("w_gla_a2", [16, 256]),
              b_gla_a=din("b_gla_a", [256]), g_gla_norm=din("g_gla_norm", [128]), w_out=din("w_out", [D, D]),
              g_mix_post=din("g_mix_post", [D]))
    out = nc.dram_tensor("out", [ntok, D], F32, kind="ExternalOutput").ap()
    x1 = nc.dram_tensor("x1_scr", [ntok, D], F32, kind="Internal").ap()
    x2 = nc.dram_tensor("x2_scr", [ntok, D], F32, kind="Internal").ap()
    nb = ntok // 128
    with ExitStack() as es:
        S = Sync(nc, es)
        C = Consts(nc, S, es)
        cur, cur_b = x, bufs(nb)
        stages = [p for p in phases]
        for i, p in enumerate(stages):
            last = (i == len(stages) - 1)
            dst = out if last else (x1 if i == 0 else x2)
            dst_b = bufs(nb)
            if p in ("ffn1", "ffn2"):
                ww = w[p]
                ffn_phase(nc, S, C, p, cur, cur_b, dst, dst_b, ww["wg"], ww["wu"], ww["wd"],
                          ww["gpre"], ww["gpost"], ntok)
            else:
                mix_phase(nc, S, C, cur, cur_b, dst, dst_b, wm, ntok)
            cur, cur_b = dst, dst_b
        S.barrier()
    return nc


_NC_CACHE = {}


def kernel(**inputs):
    names = ["g_ffn1_pre", "w_ffn1_gate", "w_ffn1_up", "w_ffn1_down", "g_ffn1_post",
             "g_ffn2_pre", "w_ffn2_gate", "w_ffn2_up", "w_ffn2_down", "g_ffn2_post",
             "g_mix_pre", "w_in", "w_gla_a2", "b_gla_a", "g_gla_norm", "w_out", "g_mix_post"]
    x = np.ascontiguousarray(np.asarray(inputs["x"], dtype=np.float32))
    shared = {n: np.ascontiguousarray(np.asarray(inputs[n], dtype=np.float32)[0]) for n in names}
    nc = build()
    in_maps = []
    for c in range(NCORES):
        m = dict(shared)
        m["x"] = x[c]
        in_maps.append(m)
    res = run_bass_kernel_spmd(nc, in_maps, core_ids=list(range(NCORES)))
    return np.stack([np.asarray(r["out"]) for r in res.results], axis=0).astype(np.float32)
```
